# Optimizing a Trainium2 kernel written in Bass

```python
import math
import jax, jax.numpy as jnp
from jax import lax
import numpy as np

D_MODEL = 1024
BATCH = 4
SEQ = 8192
DEPTH = 2

N_A_LAYERS = DEPTH // 2
N_B_LAYERS = DEPTH - N_A_LAYERS
CONV_WIDTH = 3
DIL_GROUPS = ((128, 1), (512, 4), (2048, 16))
N_ATTN_GROUPS = len(DIL_GROUPS)
HEAD_DIM = 128
HEADS_PER_GROUP = D_MODEL // HEAD_DIM
ATTN_GROUP_WIDTH = HEADS_PER_GROUP * HEAD_DIM
Q_WIDTH = N_ATTN_GROUPS * ATTN_GROUP_WIDTH
N_ATTN_HEADS = N_ATTN_GROUPS * HEADS_PER_GROUP
BLOCK = 128
NUM_BUCKETS = 32
MAX_DISTANCE = 2048
N_EXPERTS = 16
N_EXPERT_GROUPS = 4
EXPERTS_PER_GROUP = N_EXPERTS // N_EXPERT_GROUPS
TOP_K = 2
EXPERT_FF = D_MODEL // 4
ALPHA = (2 * DEPTH) ** 0.25
BETA = (8 * DEPTH) ** -0.25
LN_EPS = 1e-5
NEG_INF = -1e30

kernel_name = 'yoco_shortconv_dilated_attn_grouped_moe'


def layer_norm(x, g, b):
    xf = x.astype(jnp.float32)
    mu = jnp.mean(xf, axis=-1, keepdims=True)
    var = jnp.mean(jnp.square(xf - mu), axis=-1, keepdims=True)
    y = (xf - mu) * lax.rsqrt(var + LN_EPS) * g.astype(jnp.float32) + b.astype(jnp.float32)
    return y.astype(x.dtype)


def modulate(x, shift, scale):
    return x * (1 + scale[:, None, :]) + shift[:, None, :]


def short_conv_mixer(h, w_in, conv_w, w_out):
    bgate, cgate, v = jnp.split(h @ w_in, 3, axis=-1)
    u = cgate * v
    u = lax.conv_general_dilated(
        u, conv_w[:, None, :], window_strides=(1,), padding=[(CONV_WIDTH - 1, 0)],
        dimension_numbers=('NWC', 'WIO', 'NWC'), feature_group_count=D_MODEL)
    return (bgate * u) @ w_out


def t5_bucket(n):
    max_exact = NUM_BUCKETS // 2
    nf = jnp.maximum(n, 1).astype(jnp.float32)
    large = max_exact + (jnp.log(nf / max_exact) / math.log(MAX_DISTANCE / max_exact)
                         * (NUM_BUCKETS - max_exact)).astype(jnp.int32)
    large = jnp.minimum(large, NUM_BUCKETS - 1)
    return jnp.where(n < max_exact, n, large)


def dilated_window_attention(q, k, v, window, dilation, bias_table):
    b, s, h, hd = q.shape
    n_keys = window // dilation
    sub_len = s // dilation
    nb = -(-sub_len // BLOCK)
    lp = nb * BLOCK

    def to_sub(t):
        return t.reshape(b, sub_len, dilation, h, hd).transpose(0, 2, 1, 3, 4)

    qb = jnp.pad(to_sub(q), ((0, 0), (0, 0), (0, lp - sub_len), (0, 0), (0, 0)))
    qb = qb.reshape(b, dilation, nb, BLOCK, h, hd)

    def windows(t):
        tp = jnp.pad(to_sub(t), ((0, 0), (0, 0), (BLOCK, lp - sub_len), (0, 0), (0, 0)))
        tp = tp.reshape(b, dilation, nb + 1, BLOCK, h, hd)
        return jnp.concatenate([tp[:, :, :-1], tp[:, :, 1:]], axis=3)

    kw = windows(k)
    vw = windows(v)
    qi = jnp.arange(BLOCK)[:, None]
    kj = jnp.arange(2 * BLOCK)[None, :]
    dist = BLOCK + qi - kj
    bucket = t5_bucket(jnp.maximum(dist, 0) * dilation)
    bias = bias_table[bucket].astype(jnp.float32).transpose(2, 0, 1)
    key_idx = (jnp.arange(nb)[:, None, None] - 1) * BLOCK + kj[None]
    valid = (dist >= 0) & (dist <= n_keys) & (key_idx >= 0)

    scores = jnp.einsum('brnqhd,brnkhd->brhnqk', qb, kw).astype(jnp.float32) * (HEAD_DIM ** -0.5)
    scores = jnp.where(valid, scores + bias[:, None], NEG_INF)
    lse = jax.nn.logsumexp(scores, axis=-1)
    p = jnp.exp(scores - lse[..., None])
    out = jnp.einsum('brhnqk,brnkhd->brnqhd', p.astype(vw.dtype), vw)
    out = out.reshape(b, dilation, lp, h, hd)[:, :, :sub_len]
    out = out.transpose(0, 2, 1, 3, 4).reshape(b, s, h, hd)
    lse = lse.transpose(0, 1, 3, 4, 2).reshape(b, dilation, lp, h)[:, :, :sub_len]
    lse = lse.transpose(0, 2, 1, 3).reshape(b, s, h)
    return out, lse


def shared_kv(x, cond, kv_ada_w, kv_ada_b, w_kv):
    b, s, _ = x.shape
    shift, scale = jnp.split(cond @ kv_ada_w + kv_ada_b, 2, axis=-1)
    kv = (modulate(x, shift, scale) @ w_kv).reshape(b, s, 2, N_ATTN_GROUPS, HEADS_PER_GROUP, HEAD_DIM)
    return kv[:, :, 0], kv[:, :, 1]


def dilated_attention_mixer(h, k_sh, v_sh, w_q, w_o, rel_bias):
    b, s, _ = h.shape
    q = (h @ w_q).reshape(b, s, N_ATTN_GROUPS, HEADS_PER_GROUP, HEAD_DIM)
    outs = []
    lses = []
    for g, (window, dil) in enumerate(DIL_GROUPS):
        o, l = dilated_window_attention(
            q[:, :, g], k_sh[:, :, g], v_sh[:, :, g], window, dil,
            rel_bias[:, g * HEADS_PER_GROUP:(g + 1) * HEADS_PER_GROUP])
        outs.append(o)
        lses.append(l)
    out = jnp.stack(outs, axis=2)
    lse = jnp.stack(lses, axis=2)
    wts = jax.nn.softmax(lse, axis=2)
    mixed = jnp.sum(wts[..., None] * out.astype(jnp.float32), axis=2).astype(h.dtype)
    return mixed.reshape(b, s, ATTN_GROUP_WIDTH) @ w_o


def grouped_moe(h, router_w, router_bias, w_gate, w_up, w_down):
    b, s, _ = h.shape
    aff = jax.nn.sigmoid((h @ router_w).astype(jnp.float32))
    sel = aff + router_bias.astype(jnp.float32)
    group_score = lax.top_k(sel.reshape(b, s, N_EXPERT_GROUPS, EXPERTS_PER_GROUP), TOP_K)[0].sum(-1)
    best_group = jnp.argmax(group_score, axis=-1)
    expert_group = jnp.arange(N_EXPERTS) // EXPERTS_PER_GROUP
    masked = jnp.where(expert_group == best_group[..., None], sel, NEG_INF)
    _, idx = lax.top_k(masked, TOP_K)
    w_sel = jnp.take_along_axis(aff, idx, axis=-1)
    w_sel = w_sel / jnp.sum(w_sel, axis=-1, keepdims=True)
    combine = jnp.sum(jax.nn.one_hot(idx, N_EXPERTS, dtype=jnp.float32) * w_sel[..., None], axis=-2)
    y = jnp.zeros(h.shape, jnp.float32)
    for e in range(N_EXPERTS):
        he = jax.nn.silu(h @ w_gate[e]) * (h @ w_up[e])
        y = y + combine[..., e:e + 1] * (he @ w_down[e]).astype(jnp.float32)
    return y.astype(h.dtype)


def setup_inputs(seed: int = 0) -> dict:
    key = jax.random.key(seed)
    ks = jax.random.split(key, 20)

    def nrm(k, shape, scale):
        return jax.random.normal(k, shape, jnp.float32) * scale

    d = D_MODEL
    conv_col_scale = jnp.concatenate([jnp.ones((2 * d,), jnp.float32), jnp.full((d,), BETA, jnp.float32)])
    kv_col_scale = jnp.concatenate([jnp.ones((Q_WIDTH,), jnp.float32), jnp.full((Q_WIDTH,), BETA, jnp.float32)])
    return {
        'x': nrm(ks[0], (BATCH, SEQ, d), 1.0),
        'c': nrm(ks[1], (BATCH, d), 1.0),
        'ada_w': nrm(ks[2], (DEPTH, d, 6 * d), 0.5 * d ** -0.5),
        'ada_b': nrm(ks[3], (DEPTH, 6 * d), 0.02),
        'ln_g': 1.0 + nrm(ks[4], (DEPTH, 2, d), 0.02),
        'ln_b': nrm(ks[5], (DEPTH, 2, d), 0.02),
        'conv_w_in': nrm(ks[6], (N_A_LAYERS, d, 3 * d), d ** -0.5) * conv_col_scale,
        'conv_w': nrm(ks[7], (N_A_LAYERS, CONV_WIDTH, d), CONV_WIDTH ** -0.5),
        'conv_w_out': nrm(ks[8], (N_A_LAYERS, d, d), BETA * d ** -0.5),
        'kv_ada_w': nrm(ks[9], (d, 2 * d), 0.5 * d ** -0.5),
        'kv_ada_b': nrm(ks[10], (2 * d,), 0.02),
        'w_kv': nrm(ks[11], (d, 2 * Q_WIDTH), d ** -0.5) * kv_col_scale,
        'attn_w_q': nrm(ks[12], (N_B_LAYERS, d, Q_WIDTH), d ** -0.5),
        'attn_w_o': nrm(ks[13], (N_B_LAYERS, ATTN_GROUP_WIDTH, d), BETA * ATTN_GROUP_WIDTH ** -0.5),
        'rel_bias': nrm(ks[14], (NUM_BUCKETS, N_ATTN_HEADS), 0.2),
        'router_w': nrm(ks[15], (d, N_EXPERTS), d ** -0.5),
        'router_bias': nrm(ks[16], (N_EXPERTS,), 0.01),
        'moe_w_gate': nrm(ks[17], (DEPTH, N_EXPERTS, d, EXPERT_FF), d ** -0.5),
        'moe_w_up': nrm(ks[18], (DEPTH, N_EXPERTS, d, EXPERT_FF), BETA * d ** -0.5),
        'moe_w_down': nrm(ks[19], (DEPTH, N_EXPERTS, EXPERT_FF, d), BETA * EXPERT_FF ** -0.5),
    }


def reference(x, c, ada_w, ada_b, ln_g, ln_b, conv_w_in, conv_w, conv_w_out, kv_ada_w, kv_ada_b,
              w_kv, attn_w_q, attn_w_o, rel_bias, router_w, router_bias, moe_w_gate, moe_w_up,
              moe_w_down):
    cond = jax.nn.silu(c)
    k_sh = None
    v_sh = None
    for layer in range(DEPTH):
        sh1, sc1, g1, sh2, sc2, g2 = jnp.split(cond @ ada_w[layer] + ada_b[layer], 6, axis=-1)
        h = modulate(x, sh1, sc1)
        if layer < N_A_LAYERS:
            y = short_conv_mixer(h, conv_w_in[layer], conv_w[layer], conv_w_out[layer])
        else:
            if layer == N_A_LAYERS:
                k_sh, v_sh = shared_kv(x, cond, kv_ada_w, kv_ada_b, w_kv)
            j = layer - N_A_LAYERS
            y = dilated_attention_mixer(h, k_sh, v_sh, attn_w_q[j], attn_w_o[j], rel_bias)
        x = layer_norm(ALPHA * x + (1 + g1[:, None, :]) * y, ln_g[layer, 0], ln_b[layer, 0])
        h = modulate(x, sh2, sc2)
        y = grouped_moe(h, router_w, router_bias, moe_w_gate[layer], moe_w_up[layer], moe_w_down[layer])
        x = layer_norm(ALPHA * x + (1 + g2[:, None, :]) * y, ln_g[layer, 1], ln_b[layer, 1])
    return x
```

```python
import contextlib
import numpy as np
import ml_dtypes
import concourse.bass as bass
import concourse.mybir as mybir
from concourse.bass_utils import run_bass_kernel_spmd

F32 = mybir.dt.float32
BF16 = mybir.dt.bfloat16
ALU = mybir.AluOpType
AF = mybir.ActivationFunctionType

ENGS = ["tensor", "vector", "scalar", "gpsimd", "sync"]
EPOCH = 20000

D = 1024
KC = 8
MT = 512
NH = 4
NO = 8
NM = NH + NO
TOK = NM * MT
OWN = NO * MT
ALPHA = 4.0 ** 0.25
LN_EPS = 1e-5
NEG = -30000.0
DIL = (1, 4, 16)
NE = 16


class Trk:
    __slots__ = ("w", "r")

    def __init__(self):
        self.w = None
        self.r = {}


class Prog:
    def __init__(self, nc, sem_list):
        self.nc = nc
        self.free_sems = list(sem_list)
        self.ops = {e: [] for e in ENGS}
        self.sem = {e: self.free_sems.pop() for e in ENGS}
        self.cnt = {e: 0 for e in ENGS}
        self.pool = {"sync": [self.free_sems.pop() for _ in range(24)],
                     "gpsimd": [self.free_sems.pop() for _ in range(12)]}
        self.pool_val = {q: [0] * len(p) for q, p in self.pool.items()}
        self.rr = {q: 0 for q in self.pool}

    def _deps(self, reads, writes):
        deps = []
        for t in reads:
            if t.w is not None:
                deps.append(t.w)
        for t in writes:
            if t.w is not None:
                deps.append(t.w)
            deps.extend(t.r.values())
        return deps

    def _mark(self, token, key, reads, writes):
        for t in reads:
            t.r[key] = token
        for t in writes:
            t.w = token
            t.r = {}

    def op(self, eng, fn, reads=(), writes=(), after=()):
        deps = self._deps(reads, writes)
        for t in after:
            if t.w is not None:
                deps.append(t.w)
            deps.extend(t.r.values())
        if self.cnt[eng] >= EPOCH:
            self.sem[eng] = self.free_sems.pop()
            self.cnt[eng] = 0
        self.cnt[eng] += 1
        token = (self.sem[eng], self.cnt[eng], eng)
        self.ops[eng].append((fn, deps, (self.sem[eng], 1)))
        self._mark(token, (eng, id(self.sem[eng])), reads, writes)
        return token

    def dma(self, q, out, in_, reads=(), writes=()):
        deps = self._deps(reads, writes)
        i = self.rr[q]
        self.rr[q] = (i + 1) % len(self.pool[q])
        sem = self.pool[q][i]
        prev = self.pool_val[q][i]
        if prev > 0:
            deps.append((sem, prev, "dma"))
        self.pool_val[q][i] = prev + 16
        token = (sem, prev + 16, "dma")

        def fn(eng):
            return eng.dma_start(out=out, in_=in_)
        self.ops[q].append((fn, deps, (sem, 16)))
        self._mark(token, ("dma", id(sem)), reads, writes)
        return token

    def dma_bg(self, out, in_, reads=(), writes=()):
        return self.dma("gpsimd", out, in_, reads=reads, writes=writes)

    def replay(self, engname, eng):
        waited = {}
        for fn, deps, inc in self.ops[engname]:
            need = {}
            for (sem, val, peng) in deps:
                if engname == "tensor" and peng == "tensor":
                    continue
                k = id(sem)
                if waited.get(k, 0) >= val:
                    continue
                if k not in need or need[k][1] < val:
                    need[k] = (sem, val)
            for k, (sem, val) in need.items():
                eng.wait_ge(sem, val)
                waited[k] = val
            ins = fn(eng)
            ins.then_inc(inc[0], inc[1])
        for (sem, val) in self.barrier:
            if waited.get(id(sem), 0) < val:
                eng.wait_ge(sem, val)

    def run_block(self, final_tokens=()):
        nc = self.nc
        bar = [(self.sem[e], self.cnt[e]) for e in ENGS if self.cnt[e] > 0 and self.ops[e]]
        for q in self.pool:
            bar += [(sm, v) for sm, v in zip(self.pool[q], self.pool_val[q]) if v > 0]
        self.barrier = bar
        with nc.Block() as block:
            @block.tensor
            def _(e):
                self.replay("tensor", e)

            @block.vector
            def _(e):
                self.replay("vector", e)

            @block.scalar
            def _(e):
                self.replay("scalar", e)

            @block.gpsimd
            def _(e):
                self.replay("gpsimd", e)
                for (sem, val, _) in final_tokens:
                    e.wait_ge(sem, val)

            @block.sync
            def _(e):
                self.replay("sync", e)
                for (sem, val, _) in final_tokens:
                    e.wait_ge(sem, val)
        self.ops = {e: [] for e in ENGS}


class Builder:
    def __init__(self, phases, dbg=()):
        self.phases = phases
        self.dbg = set(dbg)
        nc = bass.Bass("TRN2", target_bir_lowering=False)
        self.nc = nc
        ext = lambda name, shape, dt=F32: nc.dram_tensor(name, shape, dt, kind="ExternalInput").ap()
        self.I = {}
        I = self.I
        I["xin"] = ext("xin", [TOK, D])
        I["xc"] = ext("xc", [128, D])
        I["flags"] = ext("flags", [128, 2])
        I["c_col"] = ext("c_col", [128, KC])
        I["ada_w"] = ext("ada_w", [2, D, 6 * D])
        I["ada_b"] = ext("ada_b", [2, 6 * D])
        I["ln_g"] = ext("ln_g", [4, 128, D])
        I["ln_b"] = ext("ln_b", [4, 128, D])
        I["conv_w_in"] = ext("conv_w_in", [D, 3 * D])
        I["conv_wc"] = ext("conv_wc", [128, KC, 3])
        I["conv_w_out"] = ext("conv_w_out", [D, D])
        I["kv_ada_w"] = ext("kv_ada_w", [D, 2 * D])
        I["kv_ada_b"] = ext("kv_ada_b", [1, 2 * D])
        I["w_kv"] = ext("w_kv", [D, 6 * D])
        I["w_q"] = ext("w_q", [D, 3 * D])
        I["w_o"] = ext("w_o", [D, D])
        I["rel_bias"] = ext("rel_bias", [32, 24])
        I["router_w"] = ext("router_w", [D, NE])
        I["router_b"] = ext("router_b", [128, NE])
        I["moe_wg"] = ext("moe_wg", [2, NE, D, 256])
        I["moe_wu"] = ext("moe_wu", [2, NE, D, 256])
        I["moe_wd"] = ext("moe_wd", [2, NE, 256, D])
        I["ident"] = ext("ident", [128, 128])
        I["antiid"] = ext("antiid", [128, 128])
        I["sel"] = ext("sel", [NE, NE * 128], BF16)
        I["oh"] = ext("oh", [3, 32, 384])
        I["mask"] = ext("mask", [1, 384])
        kind = lambda n: "ExternalOutput" if n in self.dbg else "Internal"
        dr = lambda name, shape, dt=F32: nc.dram_tensor(name, shape, dt, kind=kind(name)).ap()
        self.S = {}
        S = self.S
        S["x1s"] = dr("x1s", [TOK, D])
        S["x2s"] = dr("x2s", [TOK, D])
        S["kts"] = dr("kts", [24, 128, TOK], BF16)
        S["vs"] = dr("vs", [TOK, 3 * D], BF16)
        S["qts"] = dr("qts", [24, 128, OWN], BF16)
        S["x3s"] = dr("x3s", [OWN, D])
        S["wgu"] = dr("wgu", [2, NE, 128, KC, 512], BF16)
        S["fd"] = dr("fd", [3, 8, 384])
        S["biasd"] = dr("biasd", [24, 128, 256])
        S["gbcd"] = dr("gbcd", [4, 128, D])
        S["wdb"] = dr("wdb", [2, 128, NE, 2, D], BF16)
        S["wkvb"] = dr("wkvb", [128, KC, 6 * D], BF16)
        S["wqb"] = dr("wqb", [128, KC, 3 * D], BF16)
        S["rwb"] = dr("rwb", [128, KC, NE], BF16)
        self.t_wdb = [[Trk() for _ in range(NE)] for _ in range(2)]
        self.t_wkvb = [Trk() for _ in range(2 * KC)]
        self.t_wqb = [Trk() for _ in range(KC)]
        self.t_rwb = Trk()
        self.wgu_trk2 = [[Trk() for _ in range(NE)] for _ in range(2)]
        self.H = {}
        for nm in ["fd", "vs"]:
            self.H[nm] = S[nm].tensor
        self.out = nc.dram_tensor("out", [OWN, D], F32, kind="ExternalOutput").ap()
        self.strk = {k: [Trk() for _ in range(NM)] for k in ["x1s", "x2s", "x3s"]}
        self.strk["kts"] = [[Trk() for _ in range(3)] for _ in range(NM)]
        self.strk["qts"] = [[Trk() for _ in range(3)] for _ in range(NM)]
        self.strk["vs"] = [[Trk() for _ in range(4)] for _ in range(NM)]
        self.wgu_trk = [[Trk() for _ in range(NE)] for _ in range(2)]
        self.t_bd = [Trk() for _ in range(24)]
        self.fin = []

    def build(self):
        nc = self.nc
        with contextlib.ExitStack() as es:
            sems = [es.enter_context(nc.semaphore(f"s{i}")) for i in range(100)]
            self.P = Prog(nc, sems)
            self.banks = [es.enter_context(nc.psum_tensor(f"bank{i}", [128, 512], F32)) for i in range(8)]
            self.bank_trk = [Trk() for _ in range(8)]
            self.bank_rr = 0
            self._uid = 0

            def uname(name):
                self._uid += 1
                return f"sb{self._uid}_{name}"
            sbg = lambda name, shape, dt=F32: es.enter_context(nc.sbuf_tensor(uname(name), shape, dt))
            self.ident = sbg("ident", [128, 128]); self.t_ident = Trk()
            self.ones_f = sbg("ones_f", [128, 128]); self.t_ones_f = Trk()
            self.ones_b = sbg("ones_b", [128, 128], BF16); self.t_ones_b = Trk()
            self.flags = sbg("flags", [128, 2]); self.t_flags = Trk()
            self.adacol = sbg("adacol", [128, 2, 4, KC]); self.t_adacol = Trk()
            self.kvcol = sbg("kvcol", [128, 2, KC]); self.t_kvcol = Trk()
            self.t_gbc = [Trk() for _ in range(4)]
            P = self.P
            P.dma("sync", self.ident[:], self.I["ident"], writes=[self.t_ident])
            P.dma("sync", self.flags[:], self.I["flags"], writes=[self.t_flags])
            P.op("gpsimd", lambda e: e.memset(self.ones_f[:], 1.0), writes=[self.t_ones_f])
            P.op("gpsimd", lambda e: e.memset(self.ones_b[:], 1.0), writes=[self.t_ones_b])
            for ph in self.phases:
                with contextlib.ExitStack() as pes:
                    self.sb = lambda name, shape, dt=F32, pes=pes: pes.enter_context(nc.sbuf_tensor(uname(name), shape, dt))
                    getattr(self, "phase_" + ph)()
                    last = ph == self.phases[-1]
                    P.run_block(self.fin if last else ())
        return nc

    def bank(self):
        i = self.bank_rr
        self.bank_rr = (i + 1) % 8
        return self.banks[i], self.bank_trk[i]

    def mm(self, items, reads, writes):
        def fn(e):
            ins = None
            for (o, l, r, st, sp) in items:
                ins = e.matmul(o, lhsT=l, rhs=r, start=st, stop=sp)
            return ins
        return self.P.op("tensor", fn, reads, writes)

    def mmacc(self, out, pairs, reads, writes):
        n = len(pairs)
        return self.mm([(out, l, r, i == 0, i == n - 1) for i, (l, r) in enumerate(pairs)], reads, writes)

    def tr(self, items, reads, writes):
        ident = self.ident

        def fn(e):
            ins = None
            for (o, i) in items:
                ins = e.transpose(o, i, ident[:])
            return ins
        return self.P.op("tensor", fn, list(reads) + [self.t_ident], writes)

    def act(self, out, in_, func, reads, writes, scale=None, bias=None):
        kw = {}
        if scale is not None:
            kw["scale"] = scale
        if bias is not None:
            kw["bias"] = bias
        return self.P.op("scalar", lambda e: e.activation(out=out, in_=in_, func=func, **kw), reads, writes)

    def tt(self, eng, out, in0, in1, op, reads, writes):
        return self.P.op(eng, lambda e: e.tensor_tensor(out=out, in0=in0, in1=in1, op=op), reads, writes)

    def stt(self, eng, out, in0, scalar, in1, op0, op1, reads, writes):
        return self.P.op(eng, lambda e: e.scalar_tensor_tensor(out=out, in0=in0, scalar=scalar, in1=in1, op0=op0, op1=op1), reads, writes)

    def ts(self, eng, out, in0, s1, s2, op0, op1, reads, writes):
        if s2 is None:
            return self.P.op(eng, lambda e: e.tensor_scalar(out=out, in0=in0, scalar1=s1, scalar2=None, op0=op0), reads, writes)
        return self.P.op(eng, lambda e: e.tensor_scalar(out=out, in0=in0, scalar1=s1, scalar2=s2, op0=op0, op1=op1), reads, writes)

    def cp(self, eng, out, in_, reads, writes):
        if eng == "scalar":
            return self.P.op(eng, lambda e: e.copy(out=out, in_=in_), reads, writes)
        return self.P.op(eng, lambda e: e.tensor_copy(out=out, in_=in_), reads, writes)

    def load_gb(self, gi):
        gb = self.sb("gb", [128, D])
        t = Trk()
        self.P.dma("sync", gb[:], self.S["gbcd"][gi], reads=[self.t_gbc[gi]], writes=[t])
        return gb, t

    def load_xt(self, src, row0, xt, trk, src_trk=()):
        ap = src[row0:row0 + MT, :].rearrange("(t p) d -> p t d", p=128)
        return self.P.dma("sync", xt[:], ap, reads=list(src_trk), writes=list(trk))

    def make_hT(self, xt, xt_trk, hT, hT_trk, sc_ap, sh_ap, col_trk, ntt=4):
        for c in range(KC):
            bk, bt = self.bank()
            self.tr([(bk[:, t * 128:(t + 1) * 128], xt[:, t, c * 128:(c + 1) * 128]) for t in range(ntt)],
                    list(xt_trk)[0:ntt], [bt])
            self.act(hT[:, c, 0:ntt * 128], bk[:, 0:ntt * 128], AF.Identity, [bt, col_trk], [hT_trk],
                     scale=sc_ap[:, c:c + 1], bias=sh_ap[:, c:c + 1])

    def layer_norm(self, zz, trk, lng, lnb, lntrk, st, mv, sttrk):
        P = self.P

        def stats(e):
            e.bn_stats(out=st[:, 0, :], in_=zz[:, 0:512])
            return e.bn_stats(out=st[:, 1, :], in_=zz[:, 512:1024])
        P.op("vector", stats, [trk], [sttrk])
        P.op("vector", lambda e: e.bn_aggr(out=mv[:, 0:2], in_=st[:]), [sttrk], [sttrk])
        self.act(mv[:, 2:3], mv[:, 1:2], AF.Sqrt, [sttrk], [sttrk], scale=1.0, bias=LN_EPS)
        P.op("vector", lambda e: e.reciprocal(out=mv[:, 3:4], in_=mv[:, 2:3]), [sttrk], [sttrk])
        self.ts("vector", zz, zz, mv[:, 0:1], mv[:, 3:4], ALU.subtract, ALU.mult, [trk, sttrk], [trk])
        self.tt("vector", zz, zz, lng, ALU.mult, [trk, lntrk], [trk])
        self.tt("gpsimd", zz, zz, lnb, ALU.add, [trk, lntrk], [trk])

    def phase_setup(self):
        P, I, S = self.P, self.I, self.S
        sb = self.sb
        ccol = sb("ccol", [128, KC]); t_c = Trk()
        crep = sb("crep", [128, KC, 128]); t_crep = Trk()
        stage = [sb(f"adst{i}", [128, KC, 512]) for i in range(2)]; t_stage = [Trk(), Trk()]
        brow = [sb(f"brow{i}", [1, 512]) for i in range(2)]; t_brow = [Trk(), Trk()]
        tmp = [sb(f"adtmp{i}", [128, 512]) for i in range(2)]; t_tmp = [Trk(), Trk()]
        P.dma("sync", ccol[:], I["c_col"], writes=[t_c])
        self.act(ccol[:], ccol[:], AF.Silu, [t_c], [t_c])
        for k in range(KC):
            self.cp("vector", crep[:, k, :], ccol[:, k:k + 1].to_broadcast([128, 128]), [t_c], [t_crep])
        jobs = []
        for l in range(2):
            for j in range(12):
                jobs.append((I["ada_w"][l], I["ada_b"][l:l + 1, :], j, ("ada", l, j // 2, j % 2)))
        for j in range(4):
            jobs.append((I["kv_ada_w"], I["kv_ada_b"], j, ("kv", 0, j // 2, j % 2)))
        for n, (w, b, j, (kind, l, which, half)) in enumerate(jobs):
            s = n % 2
            P.dma("sync", stage[s][:], w[:, j * 512:(j + 1) * 512].rearrange("(k p) n -> p k n", p=128), writes=[t_stage[s]])
            P.dma("sync", brow[s][:], b[:, j * 512:(j + 1) * 512], writes=[t_brow[s]])
            bk, bt = self.bank()
            items = [(bk[:], crep[:, k, :], stage[s][:, k, :], k == 0, False) for k in range(KC)]
            items.append((bk[:], self.ones_f[0:1, :], brow[s][0:1, :], False, True))
            self.mm(items, [t_crep, t_stage[s], t_brow[s], self.t_ones_f], [bt])
            is_g = kind == "ada" and which in (2, 5)
            is_scale = (kind == "ada" and which in (1, 4)) or (kind == "kv" and which == 1)
            if is_g:
                gi = l * 2 + (0 if which == 2 else 1)
                self.act(tmp[s][:], bk[:], AF.Identity, [bt], [t_tmp[s]], scale=1.0, bias=1.0)
                P.dma("gpsimd", S["gbcd"][gi, :, half * 512:(half + 1) * 512], tmp[s][:], reads=[t_tmp[s]],
                      writes=[self.t_gbc[gi]])
            else:
                self.act(tmp[s][:], bk[:], AF.Identity, [bt], [t_tmp[s]], scale=1.0, bias=(1.0 if is_scale else 0.0))
                bk2, bt2 = self.bank()
                self.tr([(bk2[:, i * 128:(i + 1) * 128], tmp[s][:, i * 128:(i + 1) * 128]) for i in range(4)],
                        [t_tmp[s]], [bt2])
                src = bk2[:, 0:512:128]
                if kind == "ada":
                    ci = {0: 0, 1: 1, 3: 2, 4: 3}[which]
                    self.cp("vector", self.adacol[:, l, ci, half * 4:(half + 1) * 4], src, [bt2], [self.t_adacol])
                else:
                    self.cp("vector", self.kvcol[:, which, half * 4:(half + 1) * 4], src, [bt2], [self.t_kvcol])

    def phase_wconv(self):
        P, I, S = self.P, self.I, self.S
        sb = self.sb
        sg = [sb(f"wcg{i}", [128, KC, 256]) for i in range(2)]; t_sg = [Trk(), Trk()]
        su = [sb(f"wcu{i}", [128, KC, 256]) for i in range(2)]; t_su = [Trk(), Trk()]
        wb = [sb(f"wcb{i}", [128, KC, 512], BF16) for i in range(2)]; t_wb = [Trk(), Trk()]
        n = 0
        for l in range(2):
            for e in range(NE):
                s = n % 2
                n += 1
                P.dma("sync", sg[s][:], I["moe_wg"][l, e].rearrange("(k p) n -> p k n", p=128), writes=[t_sg[s]])
                P.dma("sync", su[s][:], I["moe_wu"][l, e].rearrange("(k p) n -> p k n", p=128), writes=[t_su[s]])
                self.cp("gpsimd", wb[s][:, :, 0:256], sg[s][:], [t_sg[s]], [t_wb[s]])
                self.cp("vector", wb[s][:, :, 256:512], su[s][:], [t_su[s]], [t_wb[s]])
                P.dma("gpsimd", S["wgu"][l, e], wb[s][:], reads=[t_wb[s]], writes=[self.wgu_trk[l][e]])

    def bg_jobs(self):
        I, S = self.I, self.S
        jobs = []
        kp = lambda ap: ap.rearrange("(k p) n -> p k n", p=128)

        def moe_layer(l):
            for e in range(NE):
                jobs.append((S["wgu"][l, e][:, :, 0:256], kp(I["moe_wg"][l, e]), self.wgu_trk[l][e]))
                jobs.append((S["wgu"][l, e][:, :, 256:512], kp(I["moe_wu"][l, e]), self.wgu_trk2[l][e]))
            for e in range(NE):
                jobs.append((S["wdb"][l][:, e, :, :], I["moe_wd"][l, e].rearrange("(f p) n -> p f n", p=128), self.t_wdb[l][e]))
        moe_layer(0)
        jobs.append((S["rwb"], kp(I["router_w"]), self.t_rwb))
        for k in range(KC):
            for hf in range(2):
                jobs.append((S["wkvb"][:, k, hf * 3 * D:(hf + 1) * 3 * D], I["w_kv"][k * 128:(k + 1) * 128, hf * 3 * D:(hf + 1) * 3 * D],
                             self.t_wkvb[2 * k + hf]))
        for k in range(KC):
            jobs.append((S["wqb"][:, k, :], I["w_q"][k * 128:(k + 1) * 128, :], self.t_wqb[k]))
        moe_layer(1)
        return jobs

    def phase_conv(self):
        P, I, S = self.P, self.I, self.S
        sb = self.sb
        win = sb("win", [128, KC, 3 * D], BF16); t_win = [Trk() for _ in range(2 * KC)]
        wout = sb("wout", [128, KC, D], BF16); t_wout = [Trk() for _ in range(KC)]
        wst = [sb(f"wst{i}", [128, 3 * D]) for i in range(2)]; t_wst = [Trk(), Trk()]
        cw = sb("cw", [128, KC, 3]); t_cw = Trk()
        lng = sb("lng", [128, D]); lnb = sb("lnb", [128, D]); t_ln = Trk()
        xt = [sb(f"xt{i}", [128, 4, D]) for i in range(2)]; t_xt = [[Trk() for _ in range(4)] for _ in range(2)]
        hT = [sb(f"hT{i}", [128, KC, MT], BF16) for i in range(2)]; t_hT = [Trk(), Trk()]
        zT = [sb(f"zT{i}", [128, KC, MT], BF16) for i in range(2)]; t_zT = [Trk(), Trk()]
        ub = sb("ub", [128, KC, MT + 2]); t_ub = [Trk() for _ in range(KC)]
        vs = [sb(f"vsb{i}", [128, MT]) for i in range(2)]; t_vs = [Trk(), Trk()]
        acc = [sb(f"acc{i}", [128, MT]) for i in range(2)]; t_acc = [Trk(), Trk()]
        st = sb("st", [128, 2, 6]); mv = sb("mv", [128, 4]); t_st = Trk()
        gb, t_gb = self.load_gb(0)
        P.dma("sync", cw[:], I["conv_wc"], writes=[t_cw])
        P.dma("sync", lng[:], I["ln_g"][0], writes=[t_ln])
        P.dma("sync", lnb[:], I["ln_b"][0], writes=[t_ln])
        for k in range(KC):
            s = k % 2
            P.dma("sync", wst[s][:], I["conv_w_in"][k * 128:(k + 1) * 128, :], writes=[t_wst[s]])
            self.cp("gpsimd", win[:, k, 0:1536], wst[s][:, 0:1536], [t_wst[s]], [t_win[2 * k]])
            self.cp("vector", win[:, k, 1536:3072], wst[s][:, 1536:3072], [t_wst[s]], [t_win[2 * k + 1]])
        for k in range(KC):
            s = k % 2
            P.dma("sync", wst[s][:, 0:D], I["conv_w_out"][k * 128:(k + 1) * 128, :], writes=[t_wst[s]])
            self.tt("vector" if k % 2 == 0 else "gpsimd", wout[:, k, :], wst[s][:, 0:D], gb[:], ALU.mult, [t_wst[s], t_gb], [t_wout[k]])
        sc = self.adacol[:, 0, 1, :]
        sh = self.adacol[:, 0, 0, :]
        hv = self.flags[:, 0:1]

        def proj(hTb, t_h, j, which, n):
            bk, bt = self.bank()
            col = which * D + j * 128
            self.mmacc(bk[:, 0:n], [(win[:, k, col:col + 128], hTb[:, k, 0:n]) for k in range(KC)], t_win + [t_h], [bt])
            return bk, bt

        P.dma("sync", xt[0][:, 0, :], I["xc"], writes=[t_xt[0][0]])
        self.make_hT(xt[0], t_xt[0], hT[0], t_hT[0], sc, sh, self.t_adacol, ntt=1)
        for j in range(KC):
            bc, tc = proj(hT[0], t_hT[0], j, 1, 128)
            bv, tv = proj(hT[0], t_hT[0], j, 2, 128)
            self.cp("scalar", vs[0][:, 0:128], bv[:, 0:128], [tv], [t_vs[0]])
            self.tt("vector", ub[:, j, 0:2], bc[:, 126:128], vs[0][:, 126:128], ALU.mult, [tc, t_vs[0]], [t_ub[j]])

        bgq = self.bg_jobs()
        per_tile = -(-len(bgq) // (NM - 1))
        for m in range(NM):
            s = m % 2
            self.load_xt(I["xin"], m * MT, xt[s], t_xt[s])
            self.make_hT(xt[s], t_xt[s], hT[s], t_hT[s], sc, sh, self.t_adacol)
            for _ in range(per_tile):
                if bgq:
                    o_, i_, t_ = bgq.pop(0)
                    P.dma_bg(o_, i_, writes=[t_])
            for j in range(KC):
                b = j % 2
                bB, tB = proj(hT[s], t_hT[s], j, 0, MT)
                bC, tC = proj(hT[s], t_hT[s], j, 1, MT)
                bV, tV = proj(hT[s], t_hT[s], j, 2, MT)
                self.cp("scalar", vs[b][:], bV[:], [tV], [t_vs[b]])
                self.tt("vector", ub[:, j, 2:MT + 2], bC[:], vs[b][:], ALU.mult, [tC, t_vs[b]], [t_ub[j]])
                self.act(acc[b][:], ub[:, j, 0:MT], AF.Copy, [t_ub[j], t_cw], [t_acc[b]], scale=cw[:, j, 0:1])
                self.stt("vector", acc[b][:], ub[:, j, 1:MT + 1], cw[:, j, 1:2], acc[b][:], ALU.mult, ALU.add,
                         [t_ub[j], t_cw, t_acc[b]], [t_acc[b]])
                self.stt("vector", acc[b][:], ub[:, j, 2:MT + 2], cw[:, j, 2:3], acc[b][:], ALU.mult, ALU.add,
                         [t_ub[j], t_cw, t_acc[b]], [t_acc[b]])
                self.tt("vector", zT[s][:, j, :], bB[:], acc[b][:], ALU.mult, [tB, t_acc[b]], [t_zT[s]])
                if m == NH - 1:
                    self.ts("gpsimd", ub[:, j, 0:2], ub[:, j, MT:MT + 2], hv, None, ALU.mult, None,
                            [t_ub[j], self.t_flags], [t_ub[j]])
                else:
                    self.cp("gpsimd", ub[:, j, 0:2], ub[:, j, MT:MT + 2], [t_ub[j]], [t_ub[j]])
            for t in range(4):
                for half in range(2):
                    bk, bt = self.bank()
                    self.mmacc(bk[:], [(zT[s][:, k, t * 128:(t + 1) * 128], wout[:, k, half * 512:(half + 1) * 512])
                                       for k in range(KC)], [t_zT[s]] + t_wout, [bt])
                    zz = xt[s][:, t, half * 512:(half + 1) * 512]
                    self.stt("vector", zz, zz, ALPHA, bk[:], ALU.mult, ALU.add, [t_xt[s][t], bt], [t_xt[s][t]])
                self.layer_norm(xt[s][:, t, :], t_xt[s][t], lng[:], lnb[:], t_ln, st, mv, t_st)
            P.dma("gpsimd", S["x1s"][m * MT:(m + 1) * MT, :].rearrange("(t p) d -> p t d", p=128), xt[s][:],
                  reads=t_xt[s], writes=[self.strk["x1s"][m]])

    def phase_moe0(self):
        self.moe(0, self.S["x1s"], self.strk["x1s"], self.S["x2s"], self.strk["x2s"], range(NM), 0)

    def phase_moe1(self):
        self.moe(1, self.S["x3s"], self.strk["x3s"], self.out, None, range(NO), 0)

    def moe(self, l, src, src_trk, dst, dst_trk, tiles, row_off):
        P, I, S = self.P, self.I, self.S
        sb = self.sb
        nc = self.nc
        wd = sb("wd", [128, NE, 2, D], BF16); t_wd = [Trk() for _ in range(4)]
        wgu = [sb(f"wgu{i}", [128, KC, 512], BF16) for i in range(3)]; t_wgu = [Trk() for _ in range(3)]
        rw = sb("rw", [128, KC, NE], BF16); t_rw = Trk()
        rb = sb("rb", [128, NE]); t_rb = Trk()
        sel = sb("sel", [NE, NE * 128], BF16); t_sel = Trk()
        lng = sb("lng", [128, D]); lnb = sb("lnb", [128, D]); t_ln = Trk()
        xt = [sb(f"xt{i}", [128, 4, D]) for i in range(2)]; t_xt = [[Trk() for _ in range(4)] for _ in range(2)]
        he = sb("he", [128, NE, 2, MT], BF16); t_he = [Trk() for _ in range(NE)]
        sgb = [sb(f"sgb{i}", [128, MT]) for i in range(2)]; t_sgb = [Trk(), Trk()]
        tb = [sb(f"tb{i}", [128, MT]) for i in range(2)]; t_tb = [Trk(), Trk()]
        cb = [sb(f"cb{i}", [128, MT]) for i in range(2)]; t_cb = [Trk(), Trk()]
        st = sb("st", [128, 2, 6]); mv = sb("mv", [128, 4]); t_st = Trk()
        aff = sb("aff", [128, 4, NE]); selv = sb("selv", [128, 4, NE]); msk = sb("msk", [128, 4, NE])
        eq = sb("eq", [128, 4, NE]); m1 = sb("m1", [128, 4, 4]); m2 = sb("m2", [128, 4, 4]); gs = sb("gs", [128, 4, 4])
        gm = sb("gm", [128, 4]); t1 = sb("t1", [128, 4]); comb = sb("comb", [128, 4, NE]); chi = sb("chi", [128, 4, NE], BF16)
        chi32 = sb("chi32", [128, 4, NE]); clo = sb("clo", [128, 4, NE])
        hT0 = sb("hT0", [128, KC, MT], BF16)
        cT0 = sb("cT0", [NE, 2, MT], BF16)
        t_r = Trk()
        li = 2 * l + 1
        P.dma("sync", lng[:], I["ln_g"][li], writes=[t_ln])
        P.dma("sync", lnb[:], I["ln_b"][li], writes=[t_ln])
        P.dma("sync", rb[:], I["router_b"], writes=[t_rb])
        P.dma("sync", sel[:], I["sel"], writes=[t_sel])
        gi = l * 2 + 1
        gb = sb("gb", [128, D]); t_gb = Trk()
        P.dma("sync", gb[:], S["gbcd"][gi], reads=[self.t_gbc[gi]], writes=[t_gb])
        P.dma("sync", rw[:], S["rwb"], reads=[self.t_rwb], writes=[t_rw])
        for e4 in range(4):
            P.dma("sync", wd[:, e4 * 4:(e4 + 1) * 4, :, :], S["wdb"][l][:, e4 * 4:(e4 + 1) * 4, :, :],
                  reads=self.t_wdb[l][e4 * 4:(e4 + 1) * 4], writes=[t_wd[e4]])
        hT1 = sb("hT1", [128, KC, MT], BF16)
        cT1 = sb("cT1", [NE, 2, MT], BF16)
        gtmp = [sb(f"gtmp{i}", [128, MT]) for i in range(1)]; t_gtmp = [Trk()]
        hT = [hT0, hT1]; t_hT = [Trk(), Trk()]
        cT = [cT0, cT1]; t_cT = [Trk(), Trk()]
        sc = self.adacol[:, l, 3, :]
        sh = self.adacol[:, l, 2, :]
        V = "vector"
        n_wgu = 0
        tiles = list(tiles)
        nt = len(tiles)
        pending = []

        def fetch(e):
            nonlocal n_wgu
            s = n_wgu % 3
            n_wgu += 1
            P.dma("sync", wgu[s][:], S["wgu"][l, e], reads=[self.wgu_trk[l][e], self.wgu_trk2[l][e]], writes=[t_wgu[s]])
            pending.append(s)

        def rmax(out, in_):
            return P.op(V, lambda e: e.tensor_reduce(out=out, in_=in_, axis=mybir.AxisListType.X, op=ALU.max), [t_r], [t_r])

        def front_a(mi):
            s = mi % 2
            hTb, thT = hT[s], t_hT[s]
            self.make_hT(xt[s], t_xt[s], hTb, thT, sc, sh, self.t_adacol)
            bk, bt = self.bank()
            items = []
            for t in range(4):
                for k in range(KC):
                    items.append((bk[:, t * NE:(t + 1) * NE], hTb[:, k, t * 128:(t + 1) * 128], rw[:, k, :], k == 0, k == KC - 1))
            self.mm(items, [thT, t_rw], [bt])
            lg = bk[:, 0:4 * NE].rearrange("p (t e) -> p t e", t=4)
            self.act(aff[:], lg, AF.Sigmoid, [bt], [t_r])

        def chain():
            th = []
            A = th.append
            A(lambda: self.tt(V, selv[:], aff[:], rb[:].unsqueeze(1).to_broadcast([128, 4, NE]), ALU.add, [t_r, t_rb], [t_r]))
            sel4 = selv[:].rearrange("p t (g i) -> p (t g) i", g=4)
            eq4 = eq[:].rearrange("p t (g i) -> p (t g) i", g=4)
            m1f = m1[:].rearrange("p t g -> p (t g)")
            m2f = m2[:].rearrange("p t g -> p (t g)")
            A(lambda: rmax(m1f, sel4))
            A(lambda: self.tt(V, eq4, sel4, m1f.unsqueeze(2).to_broadcast([128, 16, 4]), ALU.is_equal, [t_r], [t_r]))
            A(lambda: self.stt(V, eq4, eq4, NEG, sel4, ALU.mult, ALU.add, [t_r], [t_r]))
            A(lambda: rmax(m2f, eq4))
            A(lambda: self.tt(V, gs[:], m1[:], m2[:], ALU.add, [t_r], [t_r]))
            A(lambda: rmax(gm[:], gs[:]))
            A(lambda: self.tt(V, gs[:], gs[:], gm[:].unsqueeze(2).to_broadcast([128, 4, 4]), ALU.is_equal, [t_r], [t_r]))
            A(lambda: self.ts(V, gs[:], gs[:], -1.0, -NEG, ALU.add, ALU.mult, [t_r], [t_r]))
            gsf = gs[:].rearrange("p t g -> p (t g)")
            msk4 = msk[:].rearrange("p t (g i) -> p (t g) i", g=4)
            A(lambda: self.tt(V, msk4, sel4, gsf.unsqueeze(2).to_broadcast([128, 16, 4]), ALU.add, [t_r], [t_r]))
            A(lambda: rmax(t1[:], msk[:]))
            A(lambda: self.tt(V, eq[:], msk[:], t1[:].unsqueeze(2).to_broadcast([128, 4, NE]), ALU.is_equal, [t_r], [t_r]))
            A(lambda: self.stt(V, msk[:], eq[:], NEG, msk[:], ALU.mult, ALU.add, [t_r], [t_r]))
            A(lambda: rmax(t1[:], msk[:]))
            A(lambda: self.tt(V, selv[:], msk[:], t1[:].unsqueeze(2).to_broadcast([128, 4, NE]), ALU.is_equal, [t_r], [t_r]))
            A(lambda: self.tt(V, eq[:], eq[:], selv[:], ALU.add, [t_r], [t_r]))
            A(lambda: self.tt(V, comb[:], aff[:], eq[:], ALU.mult, [t_r], [t_r]))
            A(lambda: P.op(V, lambda e: e.tensor_reduce(out=t1[:], in_=comb[:], axis=mybir.AxisListType.X, op=ALU.add), [t_r], [t_r]))
            A(lambda: P.op(V, lambda e: e.reciprocal(out=t1[:], in_=t1[:]), [t_r], [t_r]))
            A(lambda: self.tt(V, comb[:], comb[:], t1[:].unsqueeze(2).to_broadcast([128, 4, NE]), ALU.mult, [t_r], [t_r]))
            A(lambda: self.cp(V, chi[:], comb[:], [t_r], [t_r]))
            A(lambda: self.cp(V, chi32[:], chi[:], [t_r], [t_r]))
            A(lambda: self.tt(V, clo[:], comb[:], chi32[:], ALU.subtract, [t_r], [t_r]))
            return th

        def front_c(mi):
            s = mi % 2
            cTb, tcT = cT[s], t_cT[s]
            bk, bt = self.bank()
            self.tr([(bk[0:NE, t * 128:(t + 1) * 128], chi32[:, t, :]) for t in range(4)], [t_r], [bt])
            self.cp("scalar", cTb[:, 0, :], bk[0:NE, :], [bt], [tcT])
            bk, bt = self.bank()
            self.tr([(bk[0:NE, t * 128:(t + 1) * 128], clo[:, t, :]) for t in range(4)], [t_r], [bt])
            self.cp("scalar", cTb[:, 1, :], bk[0:NE, :], [bt], [tcT])

        def expert(mi, e):
            s = mi % 2
            hTb, thT, cTb, tcT = hT[s], t_hT[s], cT[s], t_cT[s]
            if True:
                ws = pending.pop(0)
                nxt = e + 2
                if nxt < NE or mi + 1 < nt:
                    fetch(nxt % NE)
                b = e % 2
                bkc, btc = self.bank()
                self.mm([(bkc[:], sel[:, e * 128:(e + 1) * 128], cTb[:, 0, :], True, False),
                         (bkc[:], sel[:, e * 128:(e + 1) * 128], cTb[:, 1, :], False, True)], [t_sel, tcT], [btc])
                self.cp("scalar", cb[b][:], bkc[:], [btc], [t_cb[b]])
                for f in range(2):
                    bb = f
                    bkg, btg = self.bank()
                    self.mmacc(bkg[:], [(wgu[ws][:, k, f * 128:(f + 1) * 128], hTb[:, k, :]) for k in range(KC)],
                               [t_wgu[ws], thT], [btg])
                    bku, btu = self.bank()
                    self.mmacc(bku[:], [(wgu[ws][:, k, 256 + f * 128:256 + (f + 1) * 128], hTb[:, k, :]) for k in range(KC)],
                               [t_wgu[ws], thT], [btu])
                    self.act(sgb[bb][:], bkg[:], AF.Silu, [btg], [t_sgb[bb]])
                    self.tt(V, tb[bb][:], bku[:], cb[b][:], ALU.mult, [btu, t_cb[b]], [t_tb[bb]])
                    self.tt("gpsimd", he[:, e, f, :], sgb[bb][:], tb[bb][:], ALU.mult, [t_sgb[bb], t_tb[bb]], [t_he[e]])

        def back(mi):
            s = mi % 2
            m = tiles[mi]
            for t in range(4):
                for half in range(2):
                    bk, bt = self.bank()
                    pairs = []
                    for e in range(NE):
                        for f in range(2):
                            pairs.append((he[:, e, f, t * 128:(t + 1) * 128], wd[:, e, f, half * 512:(half + 1) * 512]))
                    self.mmacc(bk[:], pairs, t_he + t_wd, [bt])
                    zz = xt[s][:, t, half * 512:(half + 1) * 512]
                    gq = 0
                    self.tt(V, gtmp[gq][:], bk[:], gb[:, half * 512:(half + 1) * 512], ALU.mult, [bt, t_gb], [t_gtmp[gq]])
                    self.stt(V, zz, zz, ALPHA, gtmp[gq][:], ALU.mult, ALU.add, [t_xt[s][t], t_gtmp[gq]], [t_xt[s][t]])
                self.layer_norm(xt[s][:, t, :], t_xt[s][t], lng[:], lnb[:], t_ln, st, mv, t_st)
            tok = P.dma("gpsimd", dst[m * MT:(m + 1) * MT, :].rearrange("(t p) d -> p t d", p=128), xt[s][:],
                        reads=t_xt[s], writes=([dst_trk[m]] if dst_trk is not None else []))
            if dst_trk is None:
                self.fin.append(tok)

        def load(mi):
            m = tiles[mi]
            self.load_xt(src, m * MT, xt[mi % 2], t_xt[mi % 2], [src_trk[m]])

        load(0)
        if nt > 1:
            load(1)
        fetch(0)
        fetch(1)
        front_a(0)
        for th in chain():
            th()
        front_c(0)
        for mi in range(nt):
            cq = []
            for e in range(NE):
                if e == 5 and mi + 1 < nt:
                    front_a(mi + 1)
                    cq = chain()
                expert(mi, e)
                for _ in range(4):
                    if cq:
                        cq.pop(0)()
            while cq:
                cq.pop(0)()
            if mi + 1 < nt:
                front_c(mi + 1)
            back(mi)
            if mi + 2 < nt:
                load(mi + 2)

    def phase_kv(self):
        P, I, S = self.P, self.I, self.S
        sb = self.sb
        w = sb("wkv", [128, KC, 6 * D], BF16); t_wl = [Trk() for _ in range(KC)]
        xt = [sb(f"xt{i}", [128, 4, D]) for i in range(2)]; t_xt = [[Trk() for _ in range(4)] for _ in range(2)]
        hT = [sb(f"hT{i}", [128, KC, MT], BF16) for i in range(2)]; t_hT = [Trk(), Trk()]
        kst = [sb(f"kst{i}", [128, 8, MT], BF16) for i in range(3)]; t_kst = [[Trk() for _ in range(8)] for _ in range(3)]
        vst = [sb(f"vst{i}", [128, 3 * D], BF16) for i in range(2)]; t_vst = [[Trk() for _ in range(6)] for _ in range(2)]
        for k in range(KC):
            P.dma("sync", w[:, k, :], S["wkvb"][:, k, :], reads=self.t_wkvb[2 * k:2 * k + 2], writes=[t_wl[k]])
        sc = self.kvcol[:, 1, :]
        sh = self.kvcol[:, 0, :]
        ne = 0
        nk = 0
        nv = 0
        self.load_xt(S["x2s"], 0, xt[0], t_xt[0], [self.strk["x2s"][0]])
        for m in range(NM):
            s = m % 2
            if m + 1 < NM:
                self.load_xt(S["x2s"], (m + 1) * MT, xt[1 - s], t_xt[1 - s], [self.strk["x2s"][m + 1]])
            self.make_hT(xt[s], t_xt[s], hT[s], t_hT[s], sc, sh, self.t_kvcol)
            g2_only = m < NH - 1
            for c8 in ([2] if g2_only else range(3)):
                ks = nk % 3
                nk += 1
                for c in range(8):
                    ch = c8 * 8 + c
                    bk, bt = self.bank()
                    self.mmacc(bk[:], [(w[:, k, ch * 128:(ch + 1) * 128], hT[s][:, k, :]) for k in range(KC)], t_wl + [t_hT[s]], [bt])
                    self.cp("scalar" if ne % 2 == 0 else "vector", kst[ks][:, c, :], bk[:], [bt], [t_kst[ks][c]])
                    ne += 1
                P.dma("gpsimd", S["kts"][c8 * 8:(c8 + 1) * 8, :, m * MT:(m + 1) * MT].rearrange("c p t -> p c t"), kst[ks][:],
                      reads=t_kst[ks], writes=[self.strk["kts"][m][c8]])
            for t in range(4):
                vi = nv % 2
                nv += 1
                cbs = [4, 5] if g2_only else list(range(6))
                for cbk in cbs:
                    bk, bt = self.bank()
                    self.mmacc(bk[:], [(hT[s][:, k, t * 128:(t + 1) * 128], w[:, k, 3 * D + cbk * 512:3 * D + (cbk + 1) * 512])
                                       for k in range(KC)], t_wl + [t_hT[s]], [bt])
                    self.cp("scalar" if ne % 2 == 0 else "vector", vst[vi][:, cbk * 512:(cbk + 1) * 512], bk[:], [bt], [t_vst[vi][cbk]])
                    ne += 1
                c0 = cbs[0] * 512
                P.dma("gpsimd", S["vs"][m * MT + t * 128:m * MT + (t + 1) * 128, c0:3 * D], vst[vi][:, c0:3 * D],
                      reads=t_vst[vi], writes=[self.strk["vs"][m][t]])

    def phase_q(self):
        P, I, S = self.P, self.I, self.S
        sb = self.sb
        w = sb("wq", [128, KC, 3 * D], BF16); t_wl = [Trk() for _ in range(KC)]
        xt = [sb(f"xt{i}", [128, 4, D]) for i in range(2)]; t_xt = [[Trk() for _ in range(4)] for _ in range(2)]
        hT = [sb(f"hT{i}", [128, KC, MT], BF16) for i in range(2)]; t_hT = [Trk(), Trk()]
        qst = sb("qst", [128, 24, MT], BF16); t_qst = [Trk() for _ in range(24)]
        qs = 128.0 ** -0.5
        self.bias_tables_a()
        for k in range(KC):
            P.dma("sync", w[:, k, :], S["wqb"][:, k, :], reads=[self.t_wqb[k]], writes=[t_wl[k]])
        qcol = sb("qcol", [128, 2, KC]); t_qcol = Trk()
        self.ts("vector", qcol[:, 0, :], self.adacol[:, 1, 1, :], qs, None, ALU.mult, None, [self.t_adacol], [t_qcol])
        self.ts("vector", qcol[:, 1, :], self.adacol[:, 1, 0, :], qs, None, ALU.mult, None, [self.t_adacol], [t_qcol])
        sc = qcol[:, 0, :]
        sh = qcol[:, 1, :]
        ne = 0
        for mo in range(NO):
            m = NH + mo
            s = mo % 2
            if mo == 4:
                self.bias_tables_b()
            self.load_xt(S["x2s"], m * MT, xt[s], t_xt[s], [self.strk["x2s"][m]])
            self.make_hT(xt[s], t_xt[s], hT[s], t_hT[s], sc, sh, t_qcol)
            for ch in range(24):
                bk, bt = self.bank()
                self.mmacc(bk[:], [(w[:, k, ch * 128:(ch + 1) * 128], hT[s][:, k, :]) for k in range(KC)], t_wl + [t_hT[s]], [bt])
                self.cp("scalar" if ne % 2 == 0 else "vector", qst[:, ch, :], bk[:], [bt], [t_qst[ch]])
                ne += 1
            for c8 in range(3):
                P.dma("gpsimd", S["qts"][c8 * 8:(c8 + 1) * 8, :, mo * MT:(mo + 1) * MT].rearrange("c p t -> p c t"),
                      qst[:, c8 * 8:(c8 + 1) * 8, :], reads=t_qst[c8 * 8:(c8 + 1) * 8], writes=[self.strk["qts"][mo][c8]])

    def bias_tables_a(self):
        P, I, S = self.P, self.I, self.S
        sb = self.sb
        B = self._bt = {}
        B["anti"] = sb("anti", [128, 128]); B["t_anti"] = Trk()
        rel = sb("rel", [32, 24]); oh = sb("oh", [32, 3, 384]); mrow = sb("mrow", [1, 384]); t_rel = Trk()
        fsb = [sb(f"fsb{i}", [8, 384]) for i in range(3)]; t_fsb = [Trk() for _ in range(3)]
        B["hk"] = [sb(f"hk{i}", [128, 256]) for i in range(24)]; B["t_hk"] = [Trk() for _ in range(24)]
        B["bsb"] = [sb(f"bsb{i}", [128, 256]) for i in range(4)]; B["t_bsb"] = [Trk() for _ in range(4)]
        P.dma("gpsimd", B["anti"][:], I["antiid"], writes=[B["t_anti"]])
        P.dma("gpsimd", rel[:], I["rel_bias"], writes=[t_rel])
        P.dma("gpsimd", oh[:], I["oh"].rearrange("g b i -> b g i"), writes=[t_rel])
        P.dma("gpsimd", mrow[:], I["mask"], writes=[t_rel])
        t_fd = [Trk() for _ in range(3)]
        for g in range(3):
            bk, bt = self.bank()
            self.mm([(bk[0:8, 0:384], rel[:, g * 8:(g + 1) * 8], oh[:, g, :], True, False),
                     (bk[0:8, 0:384], self.ones_f[0:1, 0:8], mrow[0:1, :], False, True)], [t_rel, self.t_ones_f], [bt])
            self.cp("vector", fsb[g][:], bk[0:8, 0:384], [bt], [t_fsb[g]])
            P.dma("gpsimd", S["fd"][g], fsb[g][:], reads=[t_fsb[g]], writes=[t_fd[g]])
        fd_t = self.H["fd"]
        for ch in range(24):
            g, h = ch // 8, ch % 8
            src = bass.AP(fd_t, (g * 8 + h) * 384, [[1, 128], [128, 2], [1, 128]])
            P.dma("gpsimd", B["hk"][ch][:].rearrange("p (c q) -> p c q", c=2), src, reads=[t_fd[g]], writes=[B["t_hk"][ch]])

    def bias_tables_b(self):
        P, S = self.P, self.S
        B = self._bt
        for ch in range(24):
            s = ch % 4
            bk, bt = self.bank()
            self.mm([(bk[:, 0:256], B["anti"][:], B["hk"][ch][:], True, True)], [B["t_anti"], B["t_hk"][ch]], [bt])
            self.cp("vector", B["bsb"][s][:], bk[:, 0:256], [bt], [B["t_bsb"][s]])
            P.dma("gpsimd", S["biasd"][ch], B["bsb"][s][:], reads=[B["t_bsb"][s]], writes=[self.t_bd[ch]])

    def phase_attn(self):
        P, I, S = self.P, self.I, self.S
        sb = self.sb
        nc = self.nc
        wo = sb("wo", [128, KC, D], BF16); t_wo = [Trk() for _ in range(KC)]
        wst = [sb(f"wst{i}", [128, D]) for i in range(2)]; t_wst = [Trk(), Trk()]
        lng = sb("lng", [128, D]); lnb = sb("lnb", [128, D]); t_ln = Trk()
        qt = [sb(f"qt{i}", [128, 2048], BF16) for i in range(2)]; t_qt = [Trk(), Trk()]
        kt = [sb(f"kt{i}", [128, 4096], BF16) for i in range(2)]; t_kt = [Trk(), Trk()]
        vt = [sb(f"vt{i}", [128, 32, 128], BF16) for i in range(2)]; t_vt = [[Trk() for _ in range(4)] for _ in range(2)]
        bj = [sb(f"bj{i}", [128, 256]) for i in range(2)]; t_bj = [Trk(), Trk()]
        bjf = [sb(f"bjf{i}", [128, 256]) for i in range(2)]
        acc = [sb(f"acc{i}", [128, 2, 2048]) for i in range(2)]; t_accj = [[Trk()], [Trk()]]
        rcp = sb("rcp", [128, 2048]); t_rcp = Trk()
        mix = sb("mix", [128, KC, 2048], BF16); t_mix = Trk()
        tsb = [sb(f"tsb{i}", [128, 256]) for i in range(6)]; t_tsb = [Trk() for _ in range(6)]
        pT = [sb(f"pT{i}", [128, 256], BF16) for i in range(6)]; t_pT = [Trk() for _ in range(6)]
        xt = [sb(f"xt{i}", [128, 4, D]) for i in range(1)]; t_xt = [[Trk() for _ in range(4)]]
        st = sb("st", [128, 2, 6]); mv = sb("mv", [128, 4]); t_st = Trk()
        hm = self.flags[:, 1:2]
        gb, t_gb = self.load_gb(2)
        P.dma("sync", lng[:], I["ln_g"][2], writes=[t_ln])
        P.dma("sync", lnb[:], I["ln_b"][2], writes=[t_ln])
        for k in range(KC):
            s = k % 2
            P.dma("sync", wst[s][:], I["w_o"][k * 128:(k + 1) * 128, :], writes=[t_wst[s]])
            self.tt("vector" if k % 2 == 0 else "gpsimd", wo[:, k, :], wst[s][:], gb[:], ALU.mult, [t_wst[s], t_gb], [t_wo[k]])
        t_bd = self.t_bd
        LAG = 4
        jobs = [(Ssup, h, g) for Ssup in range(2) for h in range(KC) for g in range(3)]

        def job_dmas(k):
            Ssup, h, g = jobs[k]
            js = k % 2
            r = DIL[g]
            W = 128 * r
            ch = g * 8 + h
            T0 = NH * MT + Ssup * 2048
            span = W + 2048
            nchunk = span // W
            m_lo = (T0 - W) // MT
            m_hi = (T0 + 2048 - 1) // MT
            P.dma("sync", qt[js][:], S["qts"][ch, :, Ssup * 2048:(Ssup + 1) * 2048],
                  reads=[self.strk["qts"][Ssup * 4 + i][g] for i in range(4)], writes=[t_qt[js]])
            P.dma("sync", kt[js][:, 0:span], S["kts"][ch, :, T0 - W:T0 + 2048],
                  reads=[self.strk["kts"][m][g] for m in range(m_lo, m_hi + 1)], writes=[t_kt[js]])
            vs_t = self.H["vs"]
            vs_reads = [tk for m in range(m_lo, m_hi + 1) for tk in self.strk["vs"][m]]
            if r == 1:
                for c0 in range(0, nchunk, 8):
                    c1 = min(nchunk, c0 + 8)
                    src = bass.AP(vs_t, (T0 - W + 128 * c0) * 3 * D + ch * 128, [[3 * D, 128], [128 * 3 * D, c1 - c0], [1, 128]])
                    P.dma("sync", vt[js][:, c0:c1, :], src, reads=vs_reads, writes=[t_vt[js][c0 // 8]])
            else:
                step = 8 if r == 16 else 4
                part = 0
                for c in range(nchunk):
                    for rho0 in range(0, r, step):
                        src = bass.AP(vs_t, (T0 - W + rho0 + 128 * r * c) * 3 * D + ch * 128,
                                      [[r * 3 * D, 128], [3 * D, step], [1, 128]])
                        a0 = rho0 * nchunk + c
                        dstv = vt[js][:, a0:a0 + (step - 1) * nchunk + 1:nchunk, :]
                        P.dma("sync", dstv, src, reads=vs_reads, writes=[t_vt[js][part % 4]])
                        part += 1
            P.dma("sync", bj[js][:], S["biasd"][ch], reads=[t_bd[ch]], writes=[t_bj[js]])
            if Ssup == 0:
                self.cp("gpsimd", bjf[js][:, 0:128], bj[js][:, 0:128], [t_bj[js]], [t_bj[js]])
                self.ts("gpsimd", bjf[js][:, 128:256], bj[js][:, 128:256], hm, None, ALU.add, None,
                        [t_bj[js], self.t_flags], [t_bj[js]])

        pend = []
        nu = 0
        self._nev = 0
        olsb = [sb(f"olsb{i}", [128, 256]) for i in range(6)]; t_ol = [Trk() for _ in range(6)]

        def stage2(js, u4, vcur, vprv, a_out, first, t_prev, t_a):
            bkO, btO = self.bank()
            self.mm([(bkO[:, 0:128], vcur, pT[u4][:, 0:128], True, False),
                     (bkO[:, 0:128], vprv, pT[u4][:, 128:256], False, True),
                     (bkO[:, 128:256], self.ones_b[:], pT[u4][:, 0:128], True, False),
                     (bkO[:, 128:256], self.ones_b[:], pT[u4][:, 128:256], False, True)],
                    t_vt[js] + [t_pT[u4], self.t_ones_b], [btO])
            src = bkO[:, 0:256].rearrange("p (a q) -> p a q", a=2)
            ev = "scalar" if self._nev % 2 == 0 else "vector"
            self._nev += 1
            ol3 = olsb[u4][:].rearrange("p (a q) -> p a q", a=2)
            if first:
                if ev == "scalar":
                    P.op(ev, lambda e: e.copy(out=a_out, in_=src), [btO], [t_a], after=t_prev)
                else:
                    P.op(ev, lambda e: e.tensor_copy(out=a_out, in_=src), [btO], [t_a], after=t_prev)
            elif self._nev % 3 == 0:
                P.op("vector", lambda e: e.tensor_tensor(out=a_out, in0=a_out, in1=src, op=ALU.add), [btO], [t_a], after=t_prev)
            else:
                self.cp("scalar", ol3, src, [btO], [t_ol[u4]])
                P.op("gpsimd", lambda e: e.tensor_tensor(out=a_out, in0=a_out, in1=ol3, op=ALU.add), [t_ol[u4]], [t_a], after=t_prev)

        job_dmas(0)
        for k, (Ssup, h, g) in enumerate(jobs):
            js = k % 2
            r = DIL[g]
            W = 128 * r
            nchunk = (W + 2048) // W
            ai = h % 2
            nblk = 2048 // W
            ucount = 0
            t_prev = t_accj[ai]
            t_cur = []
            for n in range(nblk):
                for rho in range(r):
                    u4 = nu % 6
                    nu += 1

                    def sl(a0):
                        return slice(a0, a0 + 127 * r + 1, r) if r > 1 else slice(a0, a0 + 128)
                    qsl = sl(n * W + rho)
                    kcur = sl(W + n * W + rho)
                    kprv = sl(n * W + rho)
                    bkS, btS = self.bank()
                    self.mm([(bkS[:, 0:128], kt[js][:, kcur], qt[js][:, qsl], True, True),
                             (bkS[:, 128:256], kt[js][:, kprv], qt[js][:, qsl], True, True)],
                            [t_kt[js], t_qt[js]], [btS])
                    bias = bjf[js] if (Ssup == 0 and n == 0) else bj[js]
                    self.stt("vector", tsb[u4][:], bkS[:, 0:256], 60.0, bias[:], ALU.min, ALU.add,
                             [btS, t_bj[js]], [t_tsb[u4]])
                    self.act(pT[u4][:], tsb[u4][:], AF.Exp, [t_tsb[u4]], [t_pT[u4]])
                    t_u = Trk()
                    t_cur.append(t_u)
                    pend.append((js, u4, vt[js][:, rho * nchunk + n + 1, :], vt[js][:, rho * nchunk + n, :],
                                 acc[ai][:, :, qsl], g == 0, t_prev, t_u))
                    if len(pend) > LAG:
                        stage2(*pend.pop(0))
                    ucount += 1
                    if ucount == LAG + 1 and k + 1 < len(jobs):
                        job_dmas(k + 1)
            t_accj[ai] = t_cur
            if g == 2:
                while pend:
                    stage2(*pend.pop(0))
                t_m = Trk()
                P.op("vector", lambda e, ai=ai: e.reciprocal(out=rcp[:], in_=acc[ai][:, 1, :]), t_cur + [t_m], [t_rcp])
                self.tt("gpsimd", mix[:, h, :], acc[ai][:, 0, :], rcp[:], ALU.mult, t_cur + [t_m, t_rcp], [t_mix])
                t_accj[ai] = [t_m]
            if not (g == 2 and h == KC - 1):
                continue
            for q4 in range(4):
                m = NH + Ssup * 4 + q4
                s = 0
                self.load_xt(S["x2s"], m * MT, xt[s], t_xt[s], [self.strk["x2s"][m]])
                for t in range(4):
                    tok0 = q4 * MT + t * 128
                    for half in range(2):
                        bk, bt = self.bank()
                        self.mmacc(bk[:], [(mix[:, hh, tok0:tok0 + 128], wo[:, hh, half * 512:(half + 1) * 512]) for hh in range(KC)],
                                   [t_mix] + t_wo, [bt])
                        zz = xt[s][:, t, half * 512:(half + 1) * 512]
                        self.stt("vector", zz, zz, ALPHA, bk[:], ALU.mult, ALU.add, [t_xt[s][t], bt], [t_xt[s][t]])
                    self.layer_norm(xt[s][:, t, :], t_xt[s][t], lng[:], lnb[:], t_ln, st, mv, t_st)
                mo = Ssup * 4 + q4
                P.dma("gpsimd", S["x3s"][mo * MT:(mo + 1) * MT, :].rearrange("(t p) d -> p t d", p=128), xt[s][:],
                      reads=t_xt[s], writes=[self.strk["x3s"][mo]])


def _t5_bucket(n):
    n = np.asarray(n)
    nf = np.maximum(n, 1).astype(np.float32)
    large = 16 + (np.log(nf / np.float32(16)) / np.float32(np.log(2048 / 16)) * np.float32(16)).astype(np.int32)
    large = np.minimum(large, 31)
    return np.where(n < 16, n, large)


def _static_tables():
    oh = np.zeros((3, 32, 384), np.float32)
    mask = np.full((1, 384), NEG, np.float32)
    for g, r in enumerate(DIL):
        for i in range(384):
            dist = i - 127
            if 0 <= dist <= 128:
                b = int(_t5_bucket(np.array([dist * r]))[0])
                oh[g, b, i] = 1.0
                mask[0, i] = 0.0
    return oh, mask


def make_in_maps(inputs):
    x = np.asarray(inputs["x"], np.float32)
    oh, mask = _static_tables()
    ident = np.eye(128, dtype=np.float32)
    anti = np.ascontiguousarray(ident[::-1])
    sel = np.zeros((NE, NE * 128), np.float32)
    for e in range(NE):
        sel[e, e * 128:(e + 1) * 128] = 1.0
    sel = sel.astype(ml_dtypes.bfloat16)
    shared = {
        "ada_w": np.ascontiguousarray(inputs["ada_w"], np.float32),
        "ada_b": np.ascontiguousarray(inputs["ada_b"], np.float32),
        "ln_g": np.ascontiguousarray(np.broadcast_to(np.asarray(inputs["ln_g"], np.float32).reshape(4, 1, D), (4, 128, D))),
        "ln_b": np.ascontiguousarray(np.broadcast_to(np.asarray(inputs["ln_b"], np.float32).reshape(4, 1, D), (4, 128, D))),
        "conv_w_in": np.ascontiguousarray(inputs["conv_w_in"][0], np.float32),
        "conv_wc": np.ascontiguousarray(np.asarray(inputs["conv_w"][0], np.float32).reshape(3, KC, 128).transpose(2, 1, 0)),
        "conv_w_out": np.ascontiguousarray(inputs["conv_w_out"][0], np.float32),
        "kv_ada_w": np.ascontiguousarray(inputs["kv_ada_w"], np.float32),
        "kv_ada_b": np.ascontiguousarray(np.asarray(inputs["kv_ada_b"], np.float32).reshape(1, 2 * D)),
        "w_kv": np.ascontiguousarray(inputs["w_kv"], np.float32),
        "w_q": np.ascontiguousarray(inputs["attn_w_q"][0], np.float32),
        "w_o": np.ascontiguousarray(inputs["attn_w_o"][0], np.float32),
        "rel_bias": np.ascontiguousarray(inputs["rel_bias"], np.float32),
        "router_w": np.ascontiguousarray(inputs["router_w"], np.float32),
        "router_b": np.ascontiguousarray(np.broadcast_to(np.asarray(inputs["router_bias"], np.float32).reshape(1, NE), (128, NE))),
        "moe_wg": np.ascontiguousarray(inputs["moe_w_gate"], np.float32),
        "moe_wu": np.ascontiguousarray(inputs["moe_w_up"], np.float32),
        "moe_wd": np.ascontiguousarray(inputs["moe_w_down"], np.float32),
        "ident": ident, "antiid": anti, "sel": sel, "oh": oh, "mask": mask,
    }
    maps = []
    for i in range(8):
        b, half = i // 2, i % 2
        own0 = half * OWN
        xin = np.zeros((TOK, D), np.float32)
        xc = np.zeros((128, D), np.float32)
        if half == 1:
            xin[:] = x[b, own0 - NH * MT:own0 + OWN]
            xc[:] = x[b, own0 - NH * MT - 128:own0 - NH * MT]
        else:
            xin[NH * MT:] = x[b, 0:OWN]
        flags = np.zeros((128, 2), np.float32)
        flags[:, 0] = 1.0 if half == 1 else 0.0
        flags[:, 1] = 0.0 if half == 1 else NEG
        m = dict(shared)
        m["xin"] = xin
        m["xc"] = xc
        m["flags"] = flags
        m["c_col"] = np.ascontiguousarray(np.asarray(inputs["c"], np.float32)[b].reshape(KC, 128).T)
        maps.append(m)
    return maps


ALL_PHASES = ["setup", "conv", "moe0", "kv", "q", "attn", "moe1"]
_NC_CACHE = {}


def get_nc(phases=tuple(ALL_PHASES), dbg=()):
    key = (tuple(phases), tuple(dbg))
    if key not in _NC_CACHE:
        _NC_CACHE[key] = Builder(list(phases), dbg).build()
    return _NC_CACHE[key]


def kernel(**inputs):
    nc = get_nc()
    maps = make_in_maps(inputs)
    res = run_bass_kernel_spmd(nc, maps, core_ids=list(range(8)))
    out = np.zeros((4, 2 * OWN, D), np.float32)
    for i in range(8):
        b, half = i // 2, i % 2
        out[b, half * OWN:(half + 1) * OWN] = res.results[i]["out"]
    return out
```

```python
import contextlib
import numpy as np
import ml_dtypes
import concourse.bass as bass
import concourse.mybir as mybir
from concourse.bass_utils import run_bass_kernel_spmd

F32 = mybir.dt.float32
BF16 = mybir.dt.bfloat16
ALU = mybir.AluOpType
AF = mybir.ActivationFunctionType

ENGS = ["tensor", "vector", "scalar", "gpsimd", "sync"]
EPOCH = 20000

D = 1024
KC = 8
MT = 512
NH = 4
NO = 8
NM = NH + NO
TOK = NM * MT
OWN = NO * MT
ALPHA = 4.0 ** 0.25
LN_EPS = 1e-5
NEG = -30000.0
DIL = (1, 4, 16)
NE = 16


class Trk:
    __slots__ = ("w", "r")

    def __init__(self):
        self.w = None
        self.r = {}


class Prog:
    def __init__(self, nc, sem_list):
        self.nc = nc
        self.free_sems = list(sem_list)
        self.ops = {e: [] for e in ENGS}
        self.sem = {e: self.free_sems.pop() for e in ENGS}
        self.cnt = {e: 0 for e in ENGS}
        self.pool = {"sync": [self.free_sems.pop() for _ in range(24)],
                     "gpsimd": [self.free_sems.pop() for _ in range(12)]}
        self.pool_val = {q: [0] * len(p) for q, p in self.pool.items()}
        self.rr = {q: 0 for q in self.pool}

    def _deps(self, reads, writes):
        deps = []
        for t in reads:
            if t.w is not None:
                deps.append(t.w)
        for t in writes:
            if t.w is not None:
                deps.append(t.w)
            deps.extend(t.r.values())
        return deps

    def _mark(self, token, key, reads, writes):
        for t in reads:
            t.r[key] = token
        for t in writes:
            t.w = token
            t.r = {}

    def op(self, eng, fn, reads=(), writes=(), after=()):
        deps = self._deps(reads, writes)
        for t in after:
            if t.w is not None:
                deps.append(t.w)
            deps.extend(t.r.values())
        if self.cnt[eng] >= EPOCH:
            self.sem[eng] = self.free_sems.pop()
            self.cnt[eng] = 0
        self.cnt[eng] += 1
        token = (self.sem[eng], self.cnt[eng], eng)
        self.ops[eng].append((fn, deps, (self.sem[eng], 1)))
        self._mark(token, (eng, id(self.sem[eng])), reads, writes)
        return token

    def dma(self, q, out, in_, reads=(), writes=()):
        deps = self._deps(reads, writes)
        i = self.rr[q]
        self.rr[q] = (i + 1) % len(self.pool[q])
        sem = self.pool[q][i]
        prev = self.pool_val[q][i]
        if prev > 0:
            deps.append((sem, prev, "dma"))
        self.pool_val[q][i] = prev + 16
        token = (sem, prev + 16, "dma")

        def fn(eng):
            return eng.dma_start(out=out, in_=in_)
        self.ops[q].append((fn, deps, (sem, 16)))
        self._mark(token, ("dma", id(sem)), reads, writes)
        return token

    def dma_bg(self, out, in_, reads=(), writes=()):
        return self.dma("gpsimd", out, in_, reads=reads, writes=writes)

    def replay(self, engname, eng):
        waited = {}
        for fn, deps, inc in self.ops[engname]:
            need = {}
            for (sem, val, peng) in deps:
                if engname == "tensor" and peng == "tensor":
                    continue
                k = id(sem)
                if waited.get(k, 0) >= val:
                    continue
                if k not in need or need[k][1] < val:
                    need[k] = (sem, val)
            for k, (sem, val) in need.items():
                eng.wait_ge(sem, val)
                waited[k] = val
            ins = fn(eng)
            ins.then_inc(inc[0], inc[1])
        for (sem, val) in self.barrier:
            if waited.get(id(sem), 0) < val:
                eng.wait_ge(sem, val)

    def run_block(self, final_tokens=()):
        nc = self.nc
        bar = [(self.sem[e], self.cnt[e]) for e in ENGS if self.cnt[e] > 0 and self.ops[e]]
        for q in self.pool:
            bar += [(sm, v) for sm, v in zip(self.pool[q], self.pool_val[q]) if v > 0]
        self.barrier = bar
        with nc.Block() as block:
            @block.tensor
            def _(e):
                self.replay("tensor", e)

            @block.vector
            def _(e):
                self.replay("vector", e)

            @block.scalar
            def _(e):
                self.replay("scalar", e)

            @block.gpsimd
            def _(e):
                self.replay("gpsimd", e)
                for (sem, val, _) in final_tokens:
                    e.wait_ge(sem, val)

            @block.sync
            def _(e):
                self.replay("sync", e)
                for (sem, val, _) in final_tokens:
                    e.wait_ge(sem, val)
        self.ops = {e: [] for e in ENGS}


class Builder:
    def __init__(self, phases, dbg=()):
        self.phases = phases
        self.dbg = set(dbg)
        nc = bass.Bass("TRN2", target_bir_lowering=False)
        self.nc = nc
        ext = lambda name, shape, dt=F32: nc.dram_tensor(name, shape, dt, kind="ExternalInput").ap()
        self.I = {}
        I = self.I
        I["xin"] = ext("xin", [TOK, D])
        I["xc"] = ext("xc", [128, D])
        I["flags"] = ext("flags", [128, 2])
        I["c_col"] = ext("c_col", [128, KC])
        I["ada_w"] = ext("ada_w", [2, D, 6 * D])
        I["ada_b"] = ext("ada_b", [2, 6 * D])
        I["ln_g"] = ext("ln_g", [4, 128, D])
        I["ln_b"] = ext("ln_b", [4, 128, D])
        I["conv_w_in"] = ext("conv_w_in", [D, 3 * D])
        I["conv_wc"] = ext("conv_wc", [128, KC, 3])
        I["conv_w_out"] = ext("conv_w_out", [D, D])
        I["kv_ada_w"] = ext("kv_ada_w", [D, 2 * D])
        I["kv_ada_b"] = ext("kv_ada_b", [1, 2 * D])
        I["w_kv"] = ext("w_kv", [D, 6 * D])
        I["w_q"] = ext("w_q", [D, 3 * D])
        I["w_o"] = ext("w_o", [D, D])
        I["rel_bias"] = ext("rel_bias", [32, 24])
        I["router_w"] = ext("router_w", [D, NE])
        I["router_b"] = ext("router_b", [128, NE])
        I["moe_wg"] = ext("moe_wg", [2, NE, D, 256])
        I["moe_wu"] = ext("moe_wu", [2, NE, D, 256])
        I["moe_wd"] = ext("moe_wd", [2, NE, 256, D])
        I["ident"] = ext("ident", [128, 128])
        I["antiid"] = ext("antiid", [128, 128])
        I["sel"] = ext("sel", [NE, NE * 128], BF16)
        I["oh"] = ext("oh", [3, 32, 384])
        I["mask"] = ext("mask", [1, 384])
        kind = lambda n: "ExternalOutput" if n in self.dbg else "Internal"
        dr = lambda name, shape, dt=F32: nc.dram_tensor(name, shape, dt, kind=kind(name)).ap()
        self.S = {}
        S = self.S
        S["x1s"] = dr("x1s", [TOK, D])
        S["x2s"] = dr("x2s", [TOK, D])
        S["kts"] = dr("kts", [24, 128, TOK], BF16)
        S["vs"] = dr("vs", [TOK, 3 * D], BF16)
        S["qts"] = dr("qts", [24, 128, OWN], BF16)
        S["x3s"] = dr("x3s", [OWN, D])
        S["wgu"] = dr("wgu", [2, NE, 128, KC, 512], BF16)
        S["fd"] = dr("fd", [3, 8, 384])
        S["biasd"] = dr("biasd", [24, 128, 256])
        S["gbcd"] = dr("gbcd", [4, 128, D])
        S["wdb"] = dr("wdb", [2, 128, NE, 2, D], BF16)
        S["wkvb"] = dr("wkvb", [128, KC, 6 * D], BF16)
        S["wqb"] = dr("wqb", [128, KC, 3 * D], BF16)
        S["rwb"] = dr("rwb", [128, KC, NE], BF16)
        self.t_wdb = [[Trk() for _ in range(NE)] for _ in range(2)]
        self.t_wkvb = [Trk() for _ in range(2 * KC)]
        self.t_wqb = [Trk() for _ in range(KC)]
        self.t_rwb = Trk()
        self.wgu_trk2 = [[Trk() for _ in range(NE)] for _ in range(2)]
        self.H = {}
        for nm in ["fd", "vs"]:
            self.H[nm] = S[nm].tensor
        self.out = nc.dram_tensor("out", [OWN, D], F32, kind="ExternalOutput").ap()
        self.strk = {k: [Trk() for _ in range(NM)] for k in ["x1s", "x2s", "x3s"]}
        self.strk["kts"] = [[Trk() for _ in range(3)] for _ in range(NM)]
        self.strk["qts"] = [[Trk() for _ in range(3)] for _ in range(NM)]
        self.strk["vs"] = [[Trk() for _ in range(4)] for _ in range(NM)]
        self.wgu_trk = [[Trk() for _ in range(NE)] for _ in range(2)]
        self.t_bd = [Trk() for _ in range(24)]
        self.fin = []

    def build(self):
        nc = self.nc
        with contextlib.ExitStack() as es:
            sems = [es.enter_context(nc.semaphore(f"s{i}")) for i in range(100)]
            self.P = Prog(nc, sems)
            self.banks = [es.enter_context(nc.psum_tensor(f"bank{i}", [128, 512], F32)) for i in range(8)]
            self.bank_trk = [Trk() for _ in range(8)]
            self.bank_rr = 0
            self._uid = 0

            def uname(name):
                self._uid += 1
                return f"sb{self._uid}_{name}"
            sbg = lambda name, shape, dt=F32: es.enter_context(nc.sbuf_tensor(uname(name), shape, dt))
            self.ident = sbg("ident", [128, 128]); self.t_ident = Trk()
            self.ones_f = sbg("ones_f", [128, 128]); self.t_ones_f = Trk()
            self.ones_b = sbg("ones_b", [128, 128], BF16); self.t_ones_b = Trk()
            self.flags = sbg("flags", [128, 2]); self.t_flags = Trk()
            self.adacol = sbg("adacol", [128, 2, 4, KC]); self.t_adacol = Trk()
            self.kvcol = sbg("kvcol", [128, 2, KC]); self.t_kvcol = Trk()
            self.t_gbc = [Trk() for _ in range(4)]
            P = self.P
            P.dma("sync", self.ident[:], self.I["ident"], writes=[self.t_ident])
            P.dma("sync", self.flags[:], self.I["flags"], writes=[self.t_flags])
            P.op("gpsimd", lambda e: e.memset(self.ones_f[:], 1.0), writes=[self.t_ones_f])
            P.op("gpsimd", lambda e: e.memset(self.ones_b[:], 1.0), writes=[self.t_ones_b])
            for ph in self.phases:
                with contextlib.ExitStack() as pes:
                    self.sb = lambda name, shape, dt=F32, pes=pes: pes.enter_context(nc.sbuf_tensor(uname(name), shape, dt))
                    getattr(self, "phase_" + ph)()
                    last = ph == self.phases[-1]
                    P.run_block(self.fin if last else ())
        return nc

    def bank(self):
        i = self.bank_rr
        self.bank_rr = (i + 1) % 8
        return self.banks[i], self.bank_trk[i]

    def mm(self, items, reads, writes):
        def fn(e):
            ins = None
            for (o, l, r, st, sp) in items:
                ins = e.matmul(o, lhsT=l, rhs=r, start=st, stop=sp)
            return ins
        return self.P.op("tensor", fn, reads, writes)

    def mmacc(self, out, pairs, reads, writes):
        n = len(pairs)
        return self.mm([(out, l, r, i == 0, i == n - 1) for i, (l, r) in enumerate(pairs)], reads, writes)

    def tr(self, items, reads, writes):
        ident = self.ident

        def fn(e):
            ins = None
            for (o, i) in items:
                ins = e.transpose(o, i, ident[:])
            return ins
        return self.P.op("tensor", fn, list(reads) + [self.t_ident], writes)

    def act(self, out, in_, func, reads, writes, scale=None, bias=None):
        kw = {}
        if scale is not None:
            kw["scale"] = scale
        if bias is not None:
            kw["bias"] = bias
        return self.P.op("scalar", lambda e: e.activation(out=out, in_=in_, func=func, **kw), reads, writes)

    def tt(self, eng, out, in0, in1, op, reads, writes):
        return self.P.op(eng, lambda e: e.tensor_tensor(out=out, in0=in0, in1=in1, op=op), reads, writes)

    def stt(self, eng, out, in0, scalar, in1, op0, op1, reads, writes):
        return self.P.op(eng, lambda e: e.scalar_tensor_tensor(out=out, in0=in0, scalar=scalar, in1=in1, op0=op0, op1=op1), reads, writes)

    def ts(self, eng, out, in0, s1, s2, op0, op1, reads, writes):
        if s2 is None:
            return self.P.op(eng, lambda e: e.tensor_scalar(out=out, in0=in0, scalar1=s1, scalar2=None, op0=op0), reads, writes)
        return self.P.op(eng, lambda e: e.tensor_scalar(out=out, in0=in0, scalar1=s1, scalar2=s2, op0=op0, op1=op1), reads, writes)

    def cp(self, eng, out, in_, reads, writes):
        if eng == "scalar":
            return self.P.op(eng, lambda e: e.copy(out=out, in_=in_), reads, writes)
        return self.P.op(eng, lambda e: e.tensor_copy(out=out, in_=in_), reads, writes)

    def load_gb(self, gi):
        gb = self.sb("gb", [128, D])
        t = Trk()
        self.P.dma("sync", gb[:], self.S["gbcd"][gi], reads=[self.t_gbc[gi]], writes=[t])
        return gb, t

    def load_xt(self, src, row0, xt, trk, src_trk=()):
        ap = src[row0:row0 + MT, :].rearrange("(t p) d -> p t d", p=128)
        return self.P.dma("sync", xt[:], ap, reads=list(src_trk), writes=list(trk))

    def make_hT(self, xt, xt_trk, hT, hT_trk, sc_ap, sh_ap, col_trk, ntt=4):
        for c in range(KC):
            bk, bt = self.bank()
            self.tr([(bk[:, t * 128:(t + 1) * 128], xt[:, t, c * 128:(c + 1) * 128]) for t in range(ntt)],
                    list(xt_trk)[0:ntt], [bt])
            self.act(hT[:, c, 0:ntt * 128], bk[:, 0:ntt * 128], AF.Identity, [bt, col_trk], [hT_trk],
                     scale=sc_ap[:, c:c + 1], bias=sh_ap[:, c:c + 1])

    def layer_norm(self, zz, trk, lng, lnb, lntrk, st, mv, sttrk):
        P = self.P

        def stats(e):
            e.bn_stats(out=st[:, 0, :], in_=zz[:, 0:512])
            return e.bn_stats(out=st[:, 1, :], in_=zz[:, 512:1024])
        P.op("vector", stats, [trk], [sttrk])
        P.op("vector", lambda e: e.bn_aggr(out=mv[:, 0:2], in_=st[:]), [sttrk], [sttrk])
        self.act(mv[:, 2:3], mv[:, 1:2], AF.Sqrt, [sttrk], [sttrk], scale=1.0, bias=LN_EPS)
        P.op("vector", lambda e: e.reciprocal(out=mv[:, 3:4], in_=mv[:, 2:3]), [sttrk], [sttrk])
        self.ts("vector", zz, zz, mv[:, 0:1], mv[:, 3:4], ALU.subtract, ALU.mult, [trk, sttrk], [trk])
        self.tt("vector", zz, zz, lng, ALU.mult, [trk, lntrk], [trk])
        self.tt("gpsimd", zz, zz, lnb, ALU.add, [trk, lntrk], [trk])

    def phase_setup(self):
        P, I, S = self.P, self.I, self.S
        sb = self.sb
        ccol = sb("ccol", [128, KC]); t_c = Trk()
        crep = sb("crep", [128, KC, 128]); t_crep = Trk()
        stage = [sb(f"adst{i}", [128, KC, 512]) for i in range(2)]; t_stage = [Trk(), Trk()]
        brow = [sb(f"brow{i}", [1, 512]) for i in range(2)]; t_brow = [Trk(), Trk()]
        tmp = [sb(f"adtmp{i}", [128, 512]) for i in range(2)]; t_tmp = [Trk(), Trk()]
        P.dma("sync", ccol[:], I["c_col"], writes=[t_c])
        self.act(ccol[:], ccol[:], AF.Silu, [t_c], [t_c])
        for k in range(KC):
            self.cp("vector", crep[:, k, :], ccol[:, k:k + 1].to_broadcast([128, 128]), [t_c], [t_crep])
        jobs = []
        for l in range(2):
            for j in range(12):
                jobs.append((I["ada_w"][l], I["ada_b"][l:l + 1, :], j, ("ada", l, j // 2, j % 2)))
        for j in range(4):
            jobs.append((I["kv_ada_w"], I["kv_ada_b"], j, ("kv", 0, j // 2, j % 2)))
        for n, (w, b, j, (kind, l, which, half)) in enumerate(jobs):
            s = n % 2
            P.dma("sync", stage[s][:], w[:, j * 512:(j + 1) * 512].rearrange("(k p) n -> p k n", p=128), writes=[t_stage[s]])
            P.dma("sync", brow[s][:], b[:, j * 512:(j + 1) * 512], writes=[t_brow[s]])
            bk, bt = self.bank()
            items = [(bk[:], crep[:, k, :], stage[s][:, k, :], k == 0, False) for k in range(KC)]
            items.append((bk[:], self.ones_f[0:1, :], brow[s][0:1, :], False, True))
            self.mm(items, [t_crep, t_stage[s], t_brow[s], self.t_ones_f], [bt])
            is_g = kind == "ada" and which in (2, 5)
            is_scale = (kind == "ada" and which in (1, 4)) or (kind == "kv" and which == 1)
            if is_g:
                gi = l * 2 + (0 if which == 2 else 1)
                self.act(tmp[s][:], bk[:], AF.Identity, [bt], [t_tmp[s]], scale=1.0, bias=1.0)
                P.dma("gpsimd", S["gbcd"][gi, :, half * 512:(half + 1) * 512], tmp[s][:], reads=[t_tmp[s]],
                      writes=[self.t_gbc[gi]])
            else:
                self.act(tmp[s][:], bk[:], AF.Identity, [bt], [t_tmp[s]], scale=1.0, bias=(1.0 if is_scale else 0.0))
                bk2, bt2 = self.bank()
                self.tr([(bk2[:, i * 128:(i + 1) * 128], tmp[s][:, i * 128:(i + 1) * 128]) for i in range(4)],
                        [t_tmp[s]], [bt2])
                src = bk2[:, 0:512:128]
                if kind == "ada":
                    ci = {0: 0, 1: 1, 3: 2, 4: 3}[which]
                    self.cp("vector", self.adacol[:, l, ci, half * 4:(half + 1) * 4], src, [bt2], [self.t_adacol])
                else:
                    self.cp("vector", self.kvcol[:, which, half * 4:(half + 1) * 4], src, [bt2], [self.t_kvcol])

    def phase_wconv(self):
        P, I, S = self.P, self.I, self.S
        sb = self.sb
        sg = [sb(f"wcg{i}", [128, KC, 256]) for i in range(2)]; t_sg = [Trk(), Trk()]
        su = [sb(f"wcu{i}", [128, KC, 256]) for i in range(2)]; t_su = [Trk(), Trk()]
        wb = [sb(f"wcb{i}", [128, KC, 512], BF16) for i in range(2)]; t_wb = [Trk(), Trk()]
        n = 0
        for l in range(2):
            for e in range(NE):
                s = n % 2
                n += 1
                P.dma("sync", sg[s][:], I["moe_wg"][l, e].rearrange("(k p) n -> p k n", p=128), writes=[t_sg[s]])
                P.dma("sync", su[s][:], I["moe_wu"][l, e].rearrange("(k p) n -> p k n", p=128), writes=[t_su[s]])
                self.cp("gpsimd", wb[s][:, :, 0:256], sg[s][:], [t_sg[s]], [t_wb[s]])
                self.cp("vector", wb[s][:, :, 256:512], su[s][:], [t_su[s]], [t_wb[s]])
                P.dma("gpsimd", S["wgu"][l, e], wb[s][:], reads=[t_wb[s]], writes=[self.wgu_trk[l][e]])

    def bg_jobs(self):
        I, S = self.I, self.S
        jobs = []
        kp = lambda ap: ap.rearrange("(k p) n -> p k n", p=128)

        def moe_layer(l):
            for e in range(NE):
                jobs.append((S["wgu"][l, e][:, :, 0:256], kp(I["moe_wg"][l, e]), self.wgu_trk[l][e]))
                jobs.append((S["wgu"][l, e][:, :, 256:512], kp(I["moe_wu"][l, e]), self.wgu_trk2[l][e]))
            for e in range(NE):
                jobs.append((S["wdb"][l][:, e, :, :], I["moe_wd"][l, e].rearrange("(f p) n -> p f n", p=128), self.t_wdb[l][e]))
        moe_layer(0)
        jobs.append((S["rwb"], kp(I["router_w"]), self.t_rwb))
        for k in range(KC):
            for hf in range(2):
                jobs.append((S["wkvb"][:, k, hf * 3 * D:(hf + 1) * 3 * D], I["w_kv"][k * 128:(k + 1) * 128, hf * 3 * D:(hf + 1) * 3 * D],
                             self.t_wkvb[2 * k + hf]))
        for k in range(KC):
            jobs.append((S["wqb"][:, k, :], I["w_q"][k * 128:(k + 1) * 128, :], self.t_wqb[k]))
        moe_layer(1)
        return jobs

    def phase_conv(self):
        P, I, S = self.P, self.I, self.S
        sb = self.sb
        win = sb("win", [128, KC, 3 * D], BF16); t_win = [Trk() for _ in range(2 * KC)]
        wout = sb("wout", [128, KC, D], BF16); t_wout = [Trk() for _ in range(KC)]
        wst = [sb(f"wst{i}", [128, 3 * D]) for i in range(2)]; t_wst = [Trk(), Trk()]
        cw = sb("cw", [128, KC, 3]); t_cw = Trk()
        lng = sb("lng", [128, D]); lnb = sb("lnb", [128, D]); t_ln = Trk()
        xt = [sb(f"xt{i}", [128, 4, D]) for i in range(2)]; t_xt = [[Trk() for _ in range(4)] for _ in range(2)]
        hT = [sb(f"hT{i}", [128, KC, MT], BF16) for i in range(2)]; t_hT = [Trk(), Trk()]
        zT = [sb(f"zT{i}", [128, KC, MT], BF16) for i in range(2)]; t_zT = [Trk(), Trk()]
        ub = sb("ub", [128, KC, MT + 2]); t_ub = [Trk() for _ in range(KC)]
        vs = [sb(f"vsb{i}", [128, MT]) for i in range(2)]; t_vs = [Trk(), Trk()]
        acc = [sb(f"acc{i}", [128, MT]) for i in range(2)]; t_acc = [Trk(), Trk()]
        st = sb("st", [128, 2, 6]); mv = sb("mv", [128, 4]); t_st = Trk()
        gb, t_gb = self.load_gb(0)
        P.dma("sync", cw[:], I["conv_wc"], writes=[t_cw])
        P.dma("sync", lng[:], I["ln_g"][0], writes=[t_ln])
        P.dma("sync", lnb[:], I["ln_b"][0], writes=[t_ln])
        for k in range(KC):
            s = k % 2
            P.dma("sync", wst[s][:], I["conv_w_in"][k * 128:(k + 1) * 128, :], writes=[t_wst[s]])
            self.cp("gpsimd", win[:, k, 0:1536], wst[s][:, 0:1536], [t_wst[s]], [t_win[2 * k]])
            self.cp("vector", win[:, k, 1536:3072], wst[s][:, 1536:3072], [t_wst[s]], [t_win[2 * k + 1]])
        for k in range(KC):
            s = k % 2
            P.dma("sync", wst[s][:, 0:D], I["conv_w_out"][k * 128:(k + 1) * 128, :], writes=[t_wst[s]])
            self.tt("vector" if k % 2 == 0 else "gpsimd", wout[:, k, :], wst[s][:, 0:D], gb[:], ALU.mult, [t_wst[s], t_gb], [t_wout[k]])
        sc = self.adacol[:, 0, 1, :]
        sh = self.adacol[:, 0, 0, :]
        hv = self.flags[:, 0:1]

        def proj(hTb, t_h, j, which, n):
            bk, bt = self.bank()
            col = which * D + j * 128
            self.mmacc(bk[:, 0:n], [(win[:, k, col:col + 128], hTb[:, k, 0:n]) for k in range(KC)], t_win + [t_h], [bt])
            return bk, bt

        P.dma("sync", xt[0][:, 0, :], I["xc"], writes=[t_xt[0][0]])
        self.make_hT(xt[0], t_xt[0], hT[0], t_hT[0], sc, sh, self.t_adacol, ntt=1)
        for j in range(KC):
            bc, tc = proj(hT[0], t_hT[0], j, 1, 128)
            bv, tv = proj(hT[0], t_hT[0], j, 2, 128)
            self.cp("scalar", vs[0][:, 0:128], bv[:, 0:128], [tv], [t_vs[0]])
            self.tt("vector", ub[:, j, 0:2], bc[:, 126:128], vs[0][:, 126:128], ALU.mult, [tc, t_vs[0]], [t_ub[j]])

        bgq = self.bg_jobs()
        per_tile = -(-len(bgq) // (NM - 1))
        self.load_xt(I["xin"], 0, xt[0], t_xt[0])
        self.make_hT(xt[0], t_xt[0], hT[0], t_hT[0], sc, sh, self.t_adacol)
        for m in range(NM):
            s = m % 2
            if m + 1 < NM:
                self.load_xt(I["xin"], (m + 1) * MT, xt[1 - s], t_xt[1 - s])
            for _ in range(per_tile):
                if bgq:
                    o_, i_, t_ = bgq.pop(0)
                    P.dma_bg(o_, i_, writes=[t_])
            for j in range(KC):
                b = j % 2
                bB, tB = proj(hT[s], t_hT[s], j, 0, MT)
                bC, tC = proj(hT[s], t_hT[s], j, 1, MT)
                bV, tV = proj(hT[s], t_hT[s], j, 2, MT)
                self.cp("scalar", vs[b][:], bV[:], [tV], [t_vs[b]])
                self.tt("vector", ub[:, j, 2:MT + 2], bC[:], vs[b][:], ALU.mult, [tC, t_vs[b]], [t_ub[j]])
                self.act(acc[b][:], ub[:, j, 0:MT], AF.Copy, [t_ub[j], t_cw], [t_acc[b]], scale=cw[:, j, 0:1])
                self.stt("vector", acc[b][:], ub[:, j, 1:MT + 1], cw[:, j, 1:2], acc[b][:], ALU.mult, ALU.add,
                         [t_ub[j], t_cw, t_acc[b]], [t_acc[b]])
                self.stt("vector", acc[b][:], ub[:, j, 2:MT + 2], cw[:, j, 2:3], acc[b][:], ALU.mult, ALU.add,
                         [t_ub[j], t_cw, t_acc[b]], [t_acc[b]])
                self.tt("vector", zT[s][:, j, :], bB[:], acc[b][:], ALU.mult, [tB, t_acc[b]], [t_zT[s]])
                if m == NH - 1:
                    self.ts("gpsimd", ub[:, j, 0:2], ub[:, j, MT:MT + 2], hv, None, ALU.mult, None,
                            [t_ub[j], self.t_flags], [t_ub[j]])
                else:
                    self.cp("gpsimd", ub[:, j, 0:2], ub[:, j, MT:MT + 2], [t_ub[j]], [t_ub[j]])
            if m + 1 < NM:
                self.make_hT(xt[1 - s], t_xt[1 - s], hT[1 - s], t_hT[1 - s], sc, sh, self.t_adacol)
            for t in range(4):
                for half in range(2):
                    bk, bt = self.bank()
                    self.mmacc(bk[:], [(zT[s][:, k, t * 128:(t + 1) * 128], wout[:, k, half * 512:(half + 1) * 512])
                                       for k in range(KC)], [t_zT[s]] + t_wout, [bt])
                    zz = xt[s][:, t, half * 512:(half + 1) * 512]
                    self.stt("vector", zz, zz, ALPHA, bk[:], ALU.mult, ALU.add, [t_xt[s][t], bt], [t_xt[s][t]])
                self.layer_norm(xt[s][:, t, :], t_xt[s][t], lng[:], lnb[:], t_ln, st, mv, t_st)
            P.dma("gpsimd", S["x1s"][m * MT:(m + 1) * MT, :].rearrange("(t p) d -> p t d", p=128), xt[s][:],
                  reads=t_xt[s], writes=[self.strk["x1s"][m]])

    def phase_moe0(self):
        self.moe(0, self.S["x1s"], self.strk["x1s"], self.S["x2s"], self.strk["x2s"], range(NM), 0)

    def phase_moe1(self):
        self.moe(1, self.S["x3s"], self.strk["x3s"], self.out, None, range(NO), 0)

    def moe(self, l, src, src_trk, dst, dst_trk, tiles, row_off):
        P, I, S = self.P, self.I, self.S
        sb = self.sb
        nc = self.nc
        wd = sb("wd", [128, NE, 2, D], BF16); t_wd = [Trk() for _ in range(4)]
        wgu = [sb(f"wgu{i}", [128, KC, 512], BF16) for i in range(3)]; t_wgu = [Trk() for _ in range(3)]
        rw = sb("rw", [128, KC, NE], BF16); t_rw = Trk()
        rb = sb("rb", [128, NE]); t_rb = Trk()
        sel = sb("sel", [NE, NE * 128], BF16); t_sel = Trk()
        lng = sb("lng", [128, D]); lnb = sb("lnb", [128, D]); t_ln = Trk()
        xt = [sb(f"xt{i}", [128, 4, D]) for i in range(2)]; t_xt = [[Trk() for _ in range(4)] for _ in range(2)]
        he = sb("he", [128, NE, 2, MT], BF16); t_he = [Trk() for _ in range(NE)]
        sgb = [sb(f"sgb{i}", [128, MT]) for i in range(2)]; t_sgb = [Trk(), Trk()]
        tb = [sb(f"tb{i}", [128, MT]) for i in range(2)]; t_tb = [Trk(), Trk()]
        cb = [sb(f"cb{i}", [128, MT]) for i in range(2)]; t_cb = [Trk(), Trk()]
        st = sb("st", [128, 2, 6]); mv = sb("mv", [128, 4]); t_st = Trk()
        aff = sb("aff", [128, 4, NE]); selv = sb("selv", [128, 4, NE]); msk = sb("msk", [128, 4, NE])
        eq = sb("eq", [128, 4, NE]); m1 = sb("m1", [128, 4, 4]); m2 = sb("m2", [128, 4, 4]); gs = sb("gs", [128, 4, 4])
        gm = sb("gm", [128, 4]); t1 = sb("t1", [128, 4]); comb = sb("comb", [128, 4, NE]); chi = sb("chi", [128, 4, NE], BF16)
        chi32 = sb("chi32", [128, 4, NE]); clo = sb("clo", [128, 4, NE])
        hT0 = sb("hT0", [128, KC, MT], BF16)
        cT0 = sb("cT0", [NE, 2, MT], BF16)
        t_r = Trk()
        li = 2 * l + 1
        P.dma("sync", lng[:], I["ln_g"][li], writes=[t_ln])
        P.dma("sync", lnb[:], I["ln_b"][li], writes=[t_ln])
        P.dma("sync", rb[:], I["router_b"], writes=[t_rb])
        P.dma("sync", sel[:], I["sel"], writes=[t_sel])
        gi = l * 2 + 1
        gb = sb("gb", [128, D]); t_gb = Trk()
        P.dma("sync", gb[:], S["gbcd"][gi], reads=[self.t_gbc[gi]], writes=[t_gb])
        P.dma("sync", rw[:], S["rwb"], reads=[self.t_rwb], writes=[t_rw])
        for e4 in range(4):
            P.dma("sync", wd[:, e4 * 4:(e4 + 1) * 4, :, :], S["wdb"][l][:, e4 * 4:(e4 + 1) * 4, :, :],
                  reads=self.t_wdb[l][e4 * 4:(e4 + 1) * 4], writes=[t_wd[e4]])
        hT1 = sb("hT1", [128, KC, MT], BF16)
        cT1 = sb("cT1", [NE, 2, MT], BF16)
        gtmp = [sb(f"gtmp{i}", [128, MT]) for i in range(1)]; t_gtmp = [Trk()]
        hT = [hT0, hT1]; t_hT = [Trk(), Trk()]
        cT = [cT0, cT1]; t_cT = [Trk(), Trk()]
        sc = self.adacol[:, l, 3, :]
        sh = self.adacol[:, l, 2, :]
        V = "vector"
        n_wgu = 0
        tiles = list(tiles)
        nt = len(tiles)
        pending = []

        def fetch(e):
            nonlocal n_wgu
            s = n_wgu % 3
            n_wgu += 1
            P.dma("sync", wgu[s][:], S["wgu"][l, e], reads=[self.wgu_trk[l][e], self.wgu_trk2[l][e]], writes=[t_wgu[s]])
            pending.append(s)

        def rmax(out, in_):
            return P.op(V, lambda e: e.tensor_reduce(out=out, in_=in_, axis=mybir.AxisListType.X, op=ALU.max), [t_r], [t_r])

        def front_a(mi):
            s = mi % 2
            hTb, thT = hT[s], t_hT[s]
            self.make_hT(xt[s], t_xt[s], hTb, thT, sc, sh, self.t_adacol)
            bk, bt = self.bank()
            items = []
            for t in range(4):
                for k in range(KC):
                    items.append((bk[:, t * NE:(t + 1) * NE], hTb[:, k, t * 128:(t + 1) * 128], rw[:, k, :], k == 0, k == KC - 1))
            self.mm(items, [thT, t_rw], [bt])
            lg = bk[:, 0:4 * NE].rearrange("p (t e) -> p t e", t=4)
            self.act(aff[:], lg, AF.Sigmoid, [bt], [t_r])

        def chain():
            th = []
            A = th.append
            A(lambda: self.tt(V, selv[:], aff[:], rb[:].unsqueeze(1).to_broadcast([128, 4, NE]), ALU.add, [t_r, t_rb], [t_r]))
            sel4 = selv[:].rearrange("p t (g i) -> p (t g) i", g=4)
            eq4 = eq[:].rearrange("p t (g i) -> p (t g) i", g=4)
            m1f = m1[:].rearrange("p t g -> p (t g)")
            m2f = m2[:].rearrange("p t g -> p (t g)")
            A(lambda: rmax(m1f, sel4))
            A(lambda: self.tt(V, eq4, sel4, m1f.unsqueeze(2).to_broadcast([128, 16, 4]), ALU.is_equal, [t_r], [t_r]))
            A(lambda: self.stt(V, eq4, eq4, NEG, sel4, ALU.mult, ALU.add, [t_r], [t_r]))
            A(lambda: rmax(m2f, eq4))
            A(lambda: self.tt(V, gs[:], m1[:], m2[:], ALU.add, [t_r], [t_r]))
            A(lambda: rmax(gm[:], gs[:]))
            A(lambda: self.tt(V, gs[:], gs[:], gm[:].unsqueeze(2).to_broadcast([128, 4, 4]), ALU.is_equal, [t_r], [t_r]))
            A(lambda: self.ts(V, gs[:], gs[:], -1.0, -NEG, ALU.add, ALU.mult, [t_r], [t_r]))
            gsf = gs[:].rearrange("p t g -> p (t g)")
            msk4 = msk[:].rearrange("p t (g i) -> p (t g) i", g=4)
            A(lambda: self.tt(V, msk4, sel4, gsf.unsqueeze(2).to_broadcast([128, 16, 4]), ALU.add, [t_r], [t_r]))
            A(lambda: rmax(t1[:], msk[:]))
            A(lambda: self.tt(V, eq[:], msk[:], t1[:].unsqueeze(2).to_broadcast([128, 4, NE]), ALU.is_equal, [t_r], [t_r]))
            A(lambda: self.stt(V, msk[:], eq[:], NEG, msk[:], ALU.mult, ALU.add, [t_r], [t_r]))
            A(lambda: rmax(t1[:], msk[:]))
            A(lambda: self.tt(V, selv[:], msk[:], t1[:].unsqueeze(2).to_broadcast([128, 4, NE]), ALU.is_equal, [t_r], [t_r]))
            A(lambda: self.tt(V, eq[:], eq[:], selv[:], ALU.add, [t_r], [t_r]))
            A(lambda: self.tt(V, comb[:], aff[:], eq[:], ALU.mult, [t_r], [t_r]))
            A(lambda: P.op(V, lambda e: e.tensor_reduce(out=t1[:], in_=comb[:], axis=mybir.AxisListType.X, op=ALU.add), [t_r], [t_r]))
            A(lambda: P.op(V, lambda e: e.reciprocal(out=t1[:], in_=t1[:]), [t_r], [t_r]))
            A(lambda: self.tt(V, comb[:], comb[:], t1[:].unsqueeze(2).to_broadcast([128, 4, NE]), ALU.mult, [t_r], [t_r]))
            A(lambda: self.cp(V, chi[:], comb[:], [t_r], [t_r]))
            A(lambda: self.cp(V, chi32[:], chi[:], [t_r], [t_r]))
            A(lambda: self.tt(V, clo[:], comb[:], chi32[:], ALU.subtract, [t_r], [t_r]))
            return th

        def front_c(mi):
            s = mi % 2
            cTb, tcT = cT[s], t_cT[s]
            bk, bt = self.bank()
            self.tr([(bk[0:NE, t * 128:(t + 1) * 128], chi32[:, t, :]) for t in range(4)], [t_r], [bt])
            self.cp("scalar", cTb[:, 0, :], bk[0:NE, :], [bt], [tcT])
            bk, bt = self.bank()
            self.tr([(bk[0:NE, t * 128:(t + 1) * 128], clo[:, t, :]) for t in range(4)], [t_r], [bt])
            self.cp("scalar", cTb[:, 1, :], bk[0:NE, :], [bt], [tcT])

        def expert(mi, e):
            s = mi % 2
            hTb, thT, cTb, tcT = hT[s], t_hT[s], cT[s], t_cT[s]
            if True:
                ws = pending.pop(0)
                nxt = e + 2
                if nxt < NE or mi + 1 < nt:
                    fetch(nxt % NE)
                b = e % 2
                bkc, btc = self.bank()
                self.mm([(bkc[:], sel[:, e * 128:(e + 1) * 128], cTb[:, 0, :], True, False),
                         (bkc[:], sel[:, e * 128:(e + 1) * 128], cTb[:, 1, :], False, True)], [t_sel, tcT], [btc])
                self.cp("scalar", cb[b][:], bkc[:], [btc], [t_cb[b]])
                for f in range(2):
                    bb = f
                    bkg, btg = self.bank()
                    self.mmacc(bkg[:], [(wgu[ws][:, k, f * 128:(f + 1) * 128], hTb[:, k, :]) for k in range(KC)],
                               [t_wgu[ws], thT], [btg])
                    bku, btu = self.bank()
                    self.mmacc(bku[:], [(wgu[ws][:, k, 256 + f * 128:256 + (f + 1) * 128], hTb[:, k, :]) for k in range(KC)],
                               [t_wgu[ws], thT], [btu])
                    self.act(sgb[bb][:], bkg[:], AF.Silu, [btg], [t_sgb[bb]])
                    self.tt(V, tb[bb][:], bku[:], cb[b][:], ALU.mult, [btu, t_cb[b]], [t_tb[bb]])
                    self.tt("gpsimd", he[:, e, f, :], sgb[bb][:], tb[bb][:], ALU.mult, [t_sgb[bb], t_tb[bb]], [t_he[e]])

        def back(mi):
            s = mi % 2
            m = tiles[mi]
            for t in range(4):
                for half in range(2):
                    bk, bt = self.bank()
                    pairs = []
                    for e in range(NE):
                        for f in range(2):
                            pairs.append((he[:, e, f, t * 128:(t + 1) * 128], wd[:, e, f, half * 512:(half + 1) * 512]))
                    self.mmacc(bk[:], pairs, t_he + t_wd, [bt])
                    zz = xt[s][:, t, half * 512:(half + 1) * 512]
                    gq = 0
                    self.tt(V, gtmp[gq][:], bk[:], gb[:, half * 512:(half + 1) * 512], ALU.mult, [bt, t_gb], [t_gtmp[gq]])
                    self.stt(V, zz, zz, ALPHA, gtmp[gq][:], ALU.mult, ALU.add, [t_xt[s][t], t_gtmp[gq]], [t_xt[s][t]])
                self.layer_norm(xt[s][:, t, :], t_xt[s][t], lng[:], lnb[:], t_ln, st, mv, t_st)
            tok = P.dma("gpsimd", dst[m * MT:(m + 1) * MT, :].rearrange("(t p) d -> p t d", p=128), xt[s][:],
                        reads=t_xt[s], writes=([dst_trk[m]] if dst_trk is not None else []))
            if dst_trk is None:
                self.fin.append(tok)

        def load(mi):
            m = tiles[mi]
            self.load_xt(src, m * MT, xt[mi % 2], t_xt[mi % 2], [src_trk[m]])

        load(0)
        if nt > 1:
            load(1)
        fetch(0)
        fetch(1)
        front_a(0)
        for th in chain():
            th()
        front_c(0)
        for mi in range(nt):
            cq = []
            for e in range(NE):
                if e == 5 and mi + 1 < nt:
                    front_a(mi + 1)
                    cq = chain()
                expert(mi, e)
                for _ in range(4):
                    if cq:
                        cq.pop(0)()
            while cq:
                cq.pop(0)()
            if mi + 1 < nt:
                front_c(mi + 1)
            back(mi)
            if mi + 2 < nt:
                load(mi + 2)

    def phase_kv(self):
        P, I, S = self.P, self.I, self.S
        sb = self.sb
        w = sb("wkv", [128, KC, 6 * D], BF16); t_wl = [Trk() for _ in range(KC)]
        xt = [sb(f"xt{i}", [128, 4, D]) for i in range(2)]; t_xt = [[Trk() for _ in range(4)] for _ in range(2)]
        hT = [sb(f"hT{i}", [128, KC, MT], BF16) for i in range(2)]; t_hT = [Trk(), Trk()]
        kst = [sb(f"kst{i}", [128, 8, MT], BF16) for i in range(3)]; t_kst = [[Trk() for _ in range(8)] for _ in range(3)]
        vst = [sb(f"vst{i}", [128, 3 * D], BF16) for i in range(2)]; t_vst = [[Trk() for _ in range(6)] for _ in range(2)]
        for k in range(KC):
            P.dma("sync", w[:, k, :], S["wkvb"][:, k, :], reads=self.t_wkvb[2 * k:2 * k + 2], writes=[t_wl[k]])
        sc = self.kvcol[:, 1, :]
        sh = self.kvcol[:, 0, :]
        ne = 0
        nk = 0
        nv = 0
        self.load_xt(S["x2s"], 0, xt[0], t_xt[0], [self.strk["x2s"][0]])
        self.make_hT(xt[0], t_xt[0], hT[0], t_hT[0], sc, sh, self.t_kvcol)
        for m in range(NM):
            s = m % 2
            if m + 1 < NM:
                self.load_xt(S["x2s"], (m + 1) * MT, xt[1 - s], t_xt[1 - s], [self.strk["x2s"][m + 1]])
            g2_only = m < NH - 1
            for c8 in ([2] if g2_only else range(3)):
                ks = nk % 3
                nk += 1
                for c in range(8):
                    ch = c8 * 8 + c
                    bk, bt = self.bank()
                    self.mmacc(bk[:], [(w[:, k, ch * 128:(ch + 1) * 128], hT[s][:, k, :]) for k in range(KC)], t_wl + [t_hT[s]], [bt])
                    self.cp("scalar" if ne % 2 == 0 else "vector", kst[ks][:, c, :], bk[:], [bt], [t_kst[ks][c]])
                    ne += 1
                P.dma("gpsimd", S["kts"][c8 * 8:(c8 + 1) * 8, :, m * MT:(m + 1) * MT].rearrange("c p t -> p c t"), kst[ks][:],
                      reads=t_kst[ks], writes=[self.strk["kts"][m][c8]])
            if m + 1 < NM:
                self.make_hT(xt[1 - s], t_xt[1 - s], hT[1 - s], t_hT[1 - s], sc, sh, self.t_kvcol)
            for t in range(4):
                vi = nv % 2
                nv += 1
                cbs = [4, 5] if g2_only else list(range(6))
                for cbk in cbs:
                    bk, bt = self.bank()
                    self.mmacc(bk[:], [(hT[s][:, k, t * 128:(t + 1) * 128], w[:, k, 3 * D + cbk * 512:3 * D + (cbk + 1) * 512])
                                       for k in range(KC)], t_wl + [t_hT[s]], [bt])
                    self.cp("scalar" if ne % 2 == 0 else "vector", vst[vi][:, cbk * 512:(cbk + 1) * 512], bk[:], [bt], [t_vst[vi][cbk]])
                    ne += 1
                c0 = cbs[0] * 512
                P.dma("gpsimd", S["vs"][m * MT + t * 128:m * MT + (t + 1) * 128, c0:3 * D], vst[vi][:, c0:3 * D],
                      reads=t_vst[vi], writes=[self.strk["vs"][m][t]])

    def phase_q(self):
        P, I, S = self.P, self.I, self.S
        sb = self.sb
        w = sb("wq", [128, KC, 3 * D], BF16); t_wl = [Trk() for _ in range(KC)]
        xt = [sb(f"xt{i}", [128, 4, D]) for i in range(2)]; t_xt = [[Trk() for _ in range(4)] for _ in range(2)]
        hT = [sb(f"hT{i}", [128, KC, MT], BF16) for i in range(2)]; t_hT = [Trk(), Trk()]
        qst = sb("qst", [128, 24, MT], BF16); t_qst = [Trk() for _ in range(24)]
        qs = 128.0 ** -0.5
        self.bias_tables_a()
        for k in range(KC):
            P.dma("sync", w[:, k, :], S["wqb"][:, k, :], reads=[self.t_wqb[k]], writes=[t_wl[k]])
        qcol = sb("qcol", [128, 2, KC]); t_qcol = Trk()
        self.ts("vector", qcol[:, 0, :], self.adacol[:, 1, 1, :], qs, None, ALU.mult, None, [self.t_adacol], [t_qcol])
        self.ts("vector", qcol[:, 1, :], self.adacol[:, 1, 0, :], qs, None, ALU.mult, None, [self.t_adacol], [t_qcol])
        sc = qcol[:, 0, :]
        sh = qcol[:, 1, :]
        ne = 0
        self.load_xt(S["x2s"], NH * MT, xt[0], t_xt[0], [self.strk["x2s"][NH]])
        self.make_hT(xt[0], t_xt[0], hT[0], t_hT[0], sc, sh, t_qcol)
        for mo in range(NO):
            m = NH + mo
            s = mo % 2
            if mo == 4:
                self.bias_tables_b()
            if mo + 1 < NO:
                self.load_xt(S["x2s"], (m + 1) * MT, xt[1 - s], t_xt[1 - s], [self.strk["x2s"][m + 1]])
            for ch in range(24):
                if ch == 12 and mo + 1 < NO:
                    self.make_hT(xt[1 - s], t_xt[1 - s], hT[1 - s], t_hT[1 - s], sc, sh, t_qcol)
                bk, bt = self.bank()
                self.mmacc(bk[:], [(w[:, k, ch * 128:(ch + 1) * 128], hT[s][:, k, :]) for k in range(KC)], t_wl + [t_hT[s]], [bt])
                self.cp("scalar" if ne % 2 == 0 else "vector", qst[:, ch, :], bk[:], [bt], [t_qst[ch]])
                ne += 1
            for c8 in range(3):
                P.dma("gpsimd", S["qts"][c8 * 8:(c8 + 1) * 8, :, mo * MT:(mo + 1) * MT].rearrange("c p t -> p c t"),
                      qst[:, c8 * 8:(c8 + 1) * 8, :], reads=t_qst[c8 * 8:(c8 + 1) * 8], writes=[self.strk["qts"][mo][c8]])

    def bias_tables_a(self):
        P, I, S = self.P, self.I, self.S
        sb = self.sb
        B = self._bt = {}
        B["anti"] = sb("anti", [128, 128]); B["t_anti"] = Trk()
        rel = sb("rel", [32, 24]); oh = sb("oh", [32, 3, 384]); mrow = sb("mrow", [1, 384]); t_rel = Trk()
        fsb = [sb(f"fsb{i}", [8, 384]) for i in range(3)]; t_fsb = [Trk() for _ in range(3)]
        B["hk"] = [sb(f"hk{i}", [128, 256]) for i in range(24)]; B["t_hk"] = [Trk() for _ in range(24)]
        B["bsb"] = [sb(f"bsb{i}", [128, 256]) for i in range(4)]; B["t_bsb"] = [Trk() for _ in range(4)]
        P.dma("gpsimd", B["anti"][:], I["antiid"], writes=[B["t_anti"]])
        P.dma("gpsimd", rel[:], I["rel_bias"], writes=[t_rel])
        P.dma("gpsimd", oh[:], I["oh"].rearrange("g b i -> b g i"), writes=[t_rel])
        P.dma("gpsimd", mrow[:], I["mask"], writes=[t_rel])
        t_fd = [Trk() for _ in range(3)]
        for g in range(3):
            bk, bt = self.bank()
            self.mm([(bk[0:8, 0:384], rel[:, g * 8:(g + 1) * 8], oh[:, g, :], True, False),
                     (bk[0:8, 0:384], self.ones_f[0:1, 0:8], mrow[0:1, :], False, True)], [t_rel, self.t_ones_f], [bt])
            self.cp("vector", fsb[g][:], bk[0:8, 0:384], [bt], [t_fsb[g]])
            P.dma("gpsimd", S["fd"][g], fsb[g][:], reads=[t_fsb[g]], writes=[t_fd[g]])
        fd_t = self.H["fd"]
        for ch in range(24):
            g, h = ch // 8, ch % 8
            src = bass.AP(fd_t, (g * 8 + h) * 384, [[1, 128], [128, 2], [1, 128]])
            P.dma("gpsimd", B["hk"][ch][:].rearrange("p (c q) -> p c q", c=2), src, reads=[t_fd[g]], writes=[B["t_hk"][ch]])

    def bias_tables_b(self):
        P, S = self.P, self.S
        B = self._bt
        for ch in range(24):
            s = ch % 4
            bk, bt = self.bank()
            self.mm([(bk[:, 0:256], B["anti"][:], B["hk"][ch][:], True, True)], [B["t_anti"], B["t_hk"][ch]], [bt])
            self.cp("vector", B["bsb"][s][:], bk[:, 0:256], [bt], [B["t_bsb"][s]])
            P.dma("gpsimd", S["biasd"][ch], B["bsb"][s][:], reads=[B["t_bsb"][s]], writes=[self.t_bd[ch]])

    def phase_attn(self):
        P, I, S = self.P, self.I, self.S
        sb = self.sb
        nc = self.nc
        wo = sb("wo", [128, KC, D], BF16); t_wo = [Trk() for _ in range(KC)]
        wst = [sb(f"wst{i}", [128, D]) for i in range(2)]; t_wst = [Trk(), Trk()]
        lng = sb("lng", [128, D]); lnb = sb("lnb", [128, D]); t_ln = Trk()
        qt = [sb(f"qt{i}", [128, 2048], BF16) for i in range(2)]; t_qt = [Trk(), Trk()]
        kt = [sb(f"kt{i}", [128, 4096], BF16) for i in range(2)]; t_kt = [Trk(), Trk()]
        vt = [sb(f"vt{i}", [128, 32, 128], BF16) for i in range(2)]; t_vt = [[Trk() for _ in range(4)] for _ in range(2)]
        bj = [sb(f"bj{i}", [128, 256]) for i in range(2)]; t_bj = [Trk(), Trk()]
        bjf = [sb(f"bjf{i}", [128, 256]) for i in range(2)]
        acc = [sb(f"acc{i}", [128, 2, 2048]) for i in range(2)]; t_accj = [[Trk()], [Trk()]]
        rcp = sb("rcp", [128, 2048]); t_rcp = Trk()
        mix = sb("mix", [128, KC, 2048], BF16); t_mix = Trk()
        tsb = [sb(f"tsb{i}", [128, 256]) for i in range(6)]; t_tsb = [Trk() for _ in range(6)]
        pT = [sb(f"pT{i}", [128, 256], BF16) for i in range(6)]; t_pT = [Trk() for _ in range(6)]
        xt = [sb(f"xt{i}", [128, 4, D]) for i in range(1)]; t_xt = [[Trk() for _ in range(4)]]
        st = sb("st", [128, 2, 6]); mv = sb("mv", [128, 4]); t_st = Trk()
        hm = self.flags[:, 1:2]
        gb, t_gb = self.load_gb(2)
        P.dma("sync", lng[:], I["ln_g"][2], writes=[t_ln])
        P.dma("sync", lnb[:], I["ln_b"][2], writes=[t_ln])
        for k in range(KC):
            s = k % 2
            P.dma("sync", wst[s][:], I["w_o"][k * 128:(k + 1) * 128, :], writes=[t_wst[s]])
            self.tt("vector" if k % 2 == 0 else "gpsimd", wo[:, k, :], wst[s][:], gb[:], ALU.mult, [t_wst[s], t_gb], [t_wo[k]])
        t_bd = self.t_bd
        LAG = 4
        jobs = [(Ssup, h, g) for Ssup in range(2) for h in range(KC) for g in range(3)]

        def job_dmas(k):
            Ssup, h, g = jobs[k]
            js = k % 2
            r = DIL[g]
            W = 128 * r
            ch = g * 8 + h
            T0 = NH * MT + Ssup * 2048
            span = W + 2048
            nchunk = span // W
            m_lo = (T0 - W) // MT
            m_hi = (T0 + 2048 - 1) // MT
            P.dma("sync", qt[js][:], S["qts"][ch, :, Ssup * 2048:(Ssup + 1) * 2048],
                  reads=[self.strk["qts"][Ssup * 4 + i][g] for i in range(4)], writes=[t_qt[js]])
            P.dma("sync", kt[js][:, 0:span], S["kts"][ch, :, T0 - W:T0 + 2048],
                  reads=[self.strk["kts"][m][g] for m in range(m_lo, m_hi + 1)], writes=[t_kt[js]])
            vs_t = self.H["vs"]
            vs_reads = [tk for m in range(m_lo, m_hi + 1) for tk in self.strk["vs"][m]]
            if r == 1:
                for c0 in range(0, nchunk, 8):
                    c1 = min(nchunk, c0 + 8)
                    src = bass.AP(vs_t, (T0 - W + 128 * c0) * 3 * D + ch * 128, [[3 * D, 128], [128 * 3 * D, c1 - c0], [1, 128]])
                    P.dma("sync", vt[js][:, c0:c1, :], src, reads=vs_reads, writes=[t_vt[js][c0 // 8]])
            else:
                step = 8 if r == 16 else 4
                part = 0
                for c in range(nchunk):
                    for rho0 in range(0, r, step):
                        src = bass.AP(vs_t, (T0 - W + rho0 + 128 * r * c) * 3 * D + ch * 128,
                                      [[r * 3 * D, 128], [3 * D, step], [1, 128]])
                        a0 = rho0 * nchunk + c
                        dstv = vt[js][:, a0:a0 + (step - 1) * nchunk + 1:nchunk, :]
                        P.dma("sync", dstv, src, reads=vs_reads, writes=[t_vt[js][part % 4]])
                        part += 1
            P.dma("sync", bj[js][:], S["biasd"][ch], reads=[t_bd[ch]], writes=[t_bj[js]])
            if Ssup == 0:
                self.cp("gpsimd", bjf[js][:, 0:128], bj[js][:, 0:128], [t_bj[js]], [t_bj[js]])
                self.ts("gpsimd", bjf[js][:, 128:256], bj[js][:, 128:256], hm, None, ALU.add, None,
                        [t_bj[js], self.t_flags], [t_bj[js]])

        pend = []
        nu = 0
        self._nev = 0
        olsb = [sb(f"olsb{i}", [128, 256]) for i in range(6)]; t_ol = [Trk() for _ in range(6)]

        def stage2(js, u4, vcur, vprv, a_out, first, t_prev, t_a):
            bkO, btO = self.bank()
            self.mm([(bkO[:, 0:128], vcur, pT[u4][:, 0:128], True, False),
                     (bkO[:, 0:128], vprv, pT[u4][:, 128:256], False, True),
                     (bkO[:, 128:256], self.ones_b[:], pT[u4][:, 0:128], True, False),
                     (bkO[:, 128:256], self.ones_b[:], pT[u4][:, 128:256], False, True)],
                    t_vt[js] + [t_pT[u4], self.t_ones_b], [btO])
            src = bkO[:, 0:256].rearrange("p (a q) -> p a q", a=2)
            ev = "scalar" if self._nev % 2 == 0 else "vector"
            self._nev += 1
            ol3 = olsb[u4][:].rearrange("p (a q) -> p a q", a=2)
            if first:
                if ev == "scalar":
                    P.op(ev, lambda e: e.copy(out=a_out, in_=src), [btO], [t_a], after=t_prev)
                else:
                    P.op(ev, lambda e: e.tensor_copy(out=a_out, in_=src), [btO], [t_a], after=t_prev)
            elif self._nev % 3 == 0:
                P.op("vector", lambda e: e.tensor_tensor(out=a_out, in0=a_out, in1=src, op=ALU.add), [btO], [t_a], after=t_prev)
            else:
                self.cp("scalar", ol3, src, [btO], [t_ol[u4]])
                P.op("gpsimd", lambda e: e.tensor_tensor(out=a_out, in0=a_out, in1=ol3, op=ALU.add), [t_ol[u4]], [t_a], after=t_prev)

        job_dmas(0)
        for k, (Ssup, h, g) in enumerate(jobs):
            js = k % 2
            r = DIL[g]
            W = 128 * r
            nchunk = (W + 2048) // W
            ai = h % 2
            nblk = 2048 // W
            ucount = 0
            t_prev = t_accj[ai]
            t_cur = []
            for n in range(nblk):
                for rho in range(r):
                    u4 = nu % 6
                    nu += 1

                    def sl(a0):
                        return slice(a0, a0 + 127 * r + 1, r) if r > 1 else slice(a0, a0 + 128)
                    qsl = sl(n * W + rho)
                    kcur = sl(W + n * W + rho)
                    kprv = sl(n * W + rho)
                    bkS, btS = self.bank()
                    self.mm([(bkS[:, 0:128], kt[js][:, kcur], qt[js][:, qsl], True, True),
                             (bkS[:, 128:256], kt[js][:, kprv], qt[js][:, qsl], True, True)],
                            [t_kt[js], t_qt[js]], [btS])
                    bias = bjf[js] if (Ssup == 0 and n == 0) else bj[js]
                    self.stt("vector", tsb[u4][:], bkS[:, 0:256], 60.0, bias[:], ALU.min, ALU.add,
                             [btS, t_bj[js]], [t_tsb[u4]])
                    self.act(pT[u4][:], tsb[u4][:], AF.Exp, [t_tsb[u4]], [t_pT[u4]])
                    t_u = Trk()
                    t_cur.append(t_u)
                    pend.append((js, u4, vt[js][:, rho * nchunk + n + 1, :], vt[js][:, rho * nchunk + n, :],
                                 acc[ai][:, :, qsl], g == 0, t_prev, t_u))
                    if len(pend) > LAG:
                        stage2(*pend.pop(0))
                    ucount += 1
                    if ucount == LAG + 1 and k + 1 < len(jobs):
                        job_dmas(k + 1)
            t_accj[ai] = t_cur
            if g == 2:
                while pend:
                    stage2(*pend.pop(0))
                t_m = Trk()
                P.op("vector", lambda e, ai=ai: e.reciprocal(out=rcp[:], in_=acc[ai][:, 1, :]), t_cur + [t_m], [t_rcp])
                self.tt("gpsimd", mix[:, h, :], acc[ai][:, 0, :], rcp[:], ALU.mult, t_cur + [t_m, t_rcp], [t_mix])
                t_accj[ai] = [t_m]
            if not (g == 2 and h == KC - 1):
                continue
            for q4 in range(4):
                m = NH + Ssup * 4 + q4
                s = 0
                self.load_xt(S["x2s"], m * MT, xt[s], t_xt[s], [self.strk["x2s"][m]])
                for t in range(4):
                    tok0 = q4 * MT + t * 128
                    for half in range(2):
                        bk, bt = self.bank()
                        self.mmacc(bk[:], [(mix[:, hh, tok0:tok0 + 128], wo[:, hh, half * 512:(half + 1) * 512]) for hh in range(KC)],
                                   [t_mix] + t_wo, [bt])
                        zz = xt[s][:, t, half * 512:(half + 1) * 512]
                        self.stt("vector", zz, zz, ALPHA, bk[:], ALU.mult, ALU.add, [t_xt[s][t], bt], [t_xt[s][t]])
                    self.layer_norm(xt[s][:, t, :], t_xt[s][t], lng[:], lnb[:], t_ln, st, mv, t_st)
                mo = Ssup * 4 + q4
                P.dma("gpsimd", S["x3s"][mo * MT:(mo + 1) * MT, :].rearrange("(t p) d -> p t d", p=128), xt[s][:],
                      reads=t_xt[s], writes=[self.strk["x3s"][mo]])


def _t5_bucket(n):
    n = np.asarray(n)
    nf = np.maximum(n, 1).astype(np.float32)
    large = 16 + (np.log(nf / np.float32(16)) / np.float32(np.log(2048 / 16)) * np.float32(16)).astype(np.int32)
    large = np.minimum(large, 31)
    return np.where(n < 16, n, large)


def _static_tables():
    oh = np.zeros((3, 32, 384), np.float32)
    mask = np.full((1, 384), NEG, np.float32)
    for g, r in enumerate(DIL):
        for i in range(384):
            dist = i - 127
            if 0 <= dist <= 128:
                b = int(_t5_bucket(np.array([dist * r]))[0])
                oh[g, b, i] = 1.0
                mask[0, i] = 0.0
    return oh, mask


def make_in_maps(inputs):
    x = np.asarray(inputs["x"], np.float32)
    oh, mask = _static_tables()
    ident = np.eye(128, dtype=np.float32)
    anti = np.ascontiguousarray(ident[::-1])
    sel = np.zeros((NE, NE * 128), np.float32)
    for e in range(NE):
        sel[e, e * 128:(e + 1) * 128] = 1.0
    sel = sel.astype(ml_dtypes.bfloat16)
    shared = {
        "ada_w": np.ascontiguousarray(inputs["ada_w"], np.float32),
        "ada_b": np.ascontiguousarray(inputs["ada_b"], np.float32),
        "ln_g": np.ascontiguousarray(np.broadcast_to(np.asarray(inputs["ln_g"], np.float32).reshape(4, 1, D), (4, 128, D))),
        "ln_b": np.ascontiguousarray(np.broadcast_to(np.asarray(inputs["ln_b"], np.float32).reshape(4, 1, D), (4, 128, D))),
        "conv_w_in": np.ascontiguousarray(inputs["conv_w_in"][0], np.float32),
        "conv_wc": np.ascontiguousarray(np.asarray(inputs["conv_w"][0], np.float32).reshape(3, KC, 128).transpose(2, 1, 0)),
        "conv_w_out": np.ascontiguousarray(inputs["conv_w_out"][0], np.float32),
        "kv_ada_w": np.ascontiguousarray(inputs["kv_ada_w"], np.float32),
        "kv_ada_b": np.ascontiguousarray(np.asarray(inputs["kv_ada_b"], np.float32).reshape(1, 2 * D)),
        "w_kv": np.ascontiguousarray(inputs["w_kv"], np.float32),
        "w_q": np.ascontiguousarray(inputs["attn_w_q"][0], np.float32),
        "w_o": np.ascontiguousarray(inputs["attn_w_o"][0], np.float32),
        "rel_bias": np.ascontiguousarray(inputs["rel_bias"], np.float32),
        "router_w": np.ascontiguousarray(inputs["router_w"], np.float32),
        "router_b": np.ascontiguousarray(np.broadcast_to(np.asarray(inputs["router_bias"], np.float32).reshape(1, NE), (128, NE))),
        "moe_wg": np.ascontiguousarray(inputs["moe_w_gate"], np.float32),
        "moe_wu": np.ascontiguousarray(inputs["moe_w_up"], np.float32),
        "moe_wd": np.ascontiguousarray(inputs["moe_w_down"], np.float32),
        "ident": ident, "antiid": anti, "sel": sel, "oh": oh, "mask": mask,
    }
    maps = []
    for i in range(8):
        b, half = i // 2, i % 2
        own0 = half * OWN
        xin = np.zeros((TOK, D), np.float32)
        xc = np.zeros((128, D), np.float32)
        if half == 1:
            xin[:] = x[b, own0 - NH * MT:own0 + OWN]
            xc[:] = x[b, own0 - NH * MT - 128:own0 - NH * MT]
        else:
            xin[NH * MT:] = x[b, 0:OWN]
        flags = np.zeros((128, 2), np.float32)
        flags[:, 0] = 1.0 if half == 1 else 0.0
        flags[:, 1] = 0.0 if half == 1 else NEG
        m = dict(shared)
        m["xin"] = xin
        m["xc"] = xc
        m["flags"] = flags
        m["c_col"] = np.ascontiguousarray(np.asarray(inputs["c"], np.float32)[b].reshape(KC, 128).T)
        maps.append(m)
    return maps


ALL_PHASES = ["setup", "conv", "moe0", "kv", "q", "attn", "moe1"]
_NC_CACHE = {}


def get_nc(phases=tuple(ALL_PHASES), dbg=()):
    key = (tuple(phases), tuple(dbg))
    if key not in _NC_CACHE:
        _NC_CACHE[key] = Builder(list(phases), dbg).build()
    return _NC_CACHE[key]


def kernel(**inputs):
    nc = get_nc()
    maps = make_in_maps(inputs)
    res = run_bass_kernel_spmd(nc, maps, core_ids=list(range(8)))
    out = np.zeros((4, 2 * OWN, D), np.float32)
    for i in range(8):
        b, half = i // 2, i % 2
        out[b, half * OWN:(half + 1) * OWN] = res.results[i]["out"]
    return out
```

```python
import contextlib
import numpy as np
import ml_dtypes
import concourse.bass as bass
import concourse.mybir as mybir
from concourse.bass_utils import run_bass_kernel_spmd

F32 = mybir.dt.float32
BF16 = mybir.dt.bfloat16
ALU = mybir.AluOpType
AF = mybir.ActivationFunctionType

ENGS = ["tensor", "vector", "scalar", "gpsimd", "sync"]
EPOCH = 20000

D = 1024
KC = 8
MT = 512
NH = 4
NO = 8
NM = NH + NO
TOK = NM * MT
OWN = NO * MT
ALPHA = 4.0 ** 0.25
LN_EPS = 1e-5
NEG = -30000.0
DIL = (1, 4, 16)
NE = 16


class Trk:
    __slots__ = ("w", "r")

    def __init__(self):
        self.w = None
        self.r = {}


class Prog:
    def __init__(self, nc, sem_list):
        self.nc = nc
        self.free_sems = list(sem_list)
        self.ops = {e: [] for e in ENGS}
        self.sem = {e: self.free_sems.pop() for e in ENGS}
        self.cnt = {e: 0 for e in ENGS}
        self.pool = {"sync": [self.free_sems.pop() for _ in range(24)],
                     "gpsimd": [self.free_sems.pop() for _ in range(12)]}
        self.pool_val = {q: [0] * len(p) for q, p in self.pool.items()}
        self.rr = {q: 0 for q in self.pool}

    def _deps(self, reads, writes):
        deps = []
        for t in reads:
            if t.w is not None:
                deps.append(t.w)
        for t in writes:
            if t.w is not None:
                deps.append(t.w)
            deps.extend(t.r.values())
        return deps

    def _mark(self, token, key, reads, writes):
        for t in reads:
            t.r[key] = token
        for t in writes:
            t.w = token
            t.r = {}

    def op(self, eng, fn, reads=(), writes=(), after=()):
        deps = self._deps(reads, writes)
        for t in after:
            if t.w is not None:
                deps.append(t.w)
            deps.extend(t.r.values())
        if self.cnt[eng] >= EPOCH:
            self.sem[eng] = self.free_sems.pop()
            self.cnt[eng] = 0
        self.cnt[eng] += 1
        token = (self.sem[eng], self.cnt[eng], eng)
        self.ops[eng].append((fn, deps, (self.sem[eng], 1)))
        self._mark(token, (eng, id(self.sem[eng])), reads, writes)
        return token

    def dma(self, q, out, in_, reads=(), writes=()):
        deps = self._deps(reads, writes)
        i = self.rr[q]
        self.rr[q] = (i + 1) % len(self.pool[q])
        sem = self.pool[q][i]
        prev = self.pool_val[q][i]
        if prev > 0:
            deps.append((sem, prev, "dma"))
        self.pool_val[q][i] = prev + 16
        token = (sem, prev + 16, "dma")

        def fn(eng):
            return eng.dma_start(out=out, in_=in_)
        self.ops[q].append((fn, deps, (sem, 16)))
        self._mark(token, ("dma", id(sem)), reads, writes)
        return token

    def dma_bg(self, out, in_, reads=(), writes=()):
        return self.dma("gpsimd", out, in_, reads=reads, writes=writes)

    def replay(self, engname, eng):
        waited = {}
        for fn, deps, inc in self.ops[engname]:
            need = {}
            for (sem, val, peng) in deps:
                if engname == "tensor" and peng == "tensor":
                    continue
                k = id(sem)
                if waited.get(k, 0) >= val:
                    continue
                if k not in need or need[k][1] < val:
                    need[k] = (sem, val)
            for k, (sem, val) in need.items():
                eng.wait_ge(sem, val)
                waited[k] = val
            ins = fn(eng)
            ins.then_inc(inc[0], inc[1])
        for (sem, val) in self.barrier:
            if waited.get(id(sem), 0) < val:
                eng.wait_ge(sem, val)

    def run_block(self, final_tokens=()):
        nc = self.nc
        bar = [(self.sem[e], self.cnt[e]) for e in ENGS if self.cnt[e] > 0 and self.ops[e]]
        for q in self.pool:
            bar += [(sm, v) for sm, v in zip(self.pool[q], self.pool_val[q]) if v > 0]
        self.barrier = bar
        with nc.Block() as block:
            @block.tensor
            def _(e):
                self.replay("tensor", e)

            @block.vector
            def _(e):
                self.replay("vector", e)

            @block.scalar
            def _(e):
                self.replay("scalar", e)

            @block.gpsimd
            def _(e):
                self.replay("gpsimd", e)
                for (sem, val, _) in final_tokens:
                    e.wait_ge(sem, val)

            @block.sync
            def _(e):
                self.replay("sync", e)
                for (sem, val, _) in final_tokens:
                    e.wait_ge(sem, val)
        self.ops = {e: [] for e in ENGS}


class Builder:
    def __init__(self, phases, dbg=()):
        self.phases = phases
        self.dbg = set(dbg)
        nc = bass.Bass("TRN2", target_bir_lowering=False)
        self.nc = nc
        ext = lambda name, shape, dt=F32: nc.dram_tensor(name, shape, dt, kind="ExternalInput").ap()
        self.I = {}
        I = self.I
        I["xin"] = ext("xin", [TOK, D])
        I["xc"] = ext("xc", [128, D])
        I["flags"] = ext("flags", [128, 2])
        I["c_col"] = ext("c_col", [128, KC])
        I["ada_w"] = ext("ada_w", [2, D, 6 * D])
        I["ada_b"] = ext("ada_b", [2, 6 * D])
        I["ln_g"] = ext("ln_g", [4, 128, D])
        I["ln_b"] = ext("ln_b", [4, 128, D])
        I["conv_w_in"] = ext("conv_w_in", [D, 3 * D])
        I["conv_wc"] = ext("conv_wc", [128, KC, 3])
        I["conv_w_out"] = ext("conv_w_out", [D, D])
        I["kv_ada_w"] = ext("kv_ada_w", [D, 2 * D])
        I["kv_ada_b"] = ext("kv_ada_b", [1, 2 * D])
        I["w_kv"] = ext("w_kv", [D, 6 * D])
        I["w_q"] = ext("w_q", [D, 3 * D])
        I["w_o"] = ext("w_o", [D, D])
        I["rel_bias"] = ext("rel_bias", [32, 24])
        I["router_w"] = ext("router_w", [D, NE])
        I["router_b"] = ext("router_b", [128, NE])
        I["moe_wg"] = ext("moe_wg", [2, NE, D, 256])
        I["moe_wu"] = ext("moe_wu", [2, NE, D, 256])
        I["moe_wd"] = ext("moe_wd", [2, NE, 256, D])
        I["ident"] = ext("ident", [128, 128])
        I["antiid"] = ext("antiid", [128, 128])
        I["sel"] = ext("sel", [NE, NE * 128], BF16)
        I["oh"] = ext("oh", [3, 32, 384])
        I["mask"] = ext("mask", [1, 384])
        kind = lambda n: "ExternalOutput" if n in self.dbg else "Internal"
        dr = lambda name, shape, dt=F32: nc.dram_tensor(name, shape, dt, kind=kind(name)).ap()
        self.S = {}
        S = self.S
        S["x1s"] = dr("x1s", [TOK, D])
        S["x2s"] = dr("x2s", [TOK, D])
        S["kts"] = dr("kts", [24, 128, TOK], BF16)
        S["vs"] = dr("vs", [TOK, 3 * D], BF16)
        S["qts"] = dr("qts", [24, 128, OWN], BF16)
        S["x3s"] = dr("x3s", [OWN, D])
        S["wgu"] = dr("wgu", [2, NE, 128, KC, 512], BF16)
        S["fd"] = dr("fd", [3, 8, 384])
        S["biasd"] = dr("biasd", [24, 128, 256])
        S["gbcd"] = dr("gbcd", [4, 128, D])
        S["wdb"] = dr("wdb", [2, 128, NE, 2, D], BF16)
        S["wkvb"] = dr("wkvb", [128, KC, 6 * D], BF16)
        S["wqb"] = dr("wqb", [128, KC, 3 * D], BF16)
        S["rwb"] = dr("rwb", [128, KC, NE], BF16)
        S["cbd"] = dr("cbd", [2, 2, NE, MT])
        self.t_wdb = [[Trk() for _ in range(NE)] for _ in range(2)]
        self.t_wkvb = [Trk() for _ in range(2 * KC)]
        self.t_wqb = [Trk() for _ in range(KC)]
        self.t_rwb = Trk()
        self.wgu_trk2 = [[Trk() for _ in range(NE)] for _ in range(2)]
        self.H = {}
        for nm in ["fd", "vs"]:
            self.H[nm] = S[nm].tensor
        self.out = nc.dram_tensor("out", [OWN, D], F32, kind="ExternalOutput").ap()
        self.strk = {k: [Trk() for _ in range(NM)] for k in ["x1s", "x2s", "x3s"]}
        self.strk["kts"] = [[Trk() for _ in range(3)] for _ in range(NM)]
        self.strk["qts"] = [[Trk() for _ in range(3)] for _ in range(NM)]
        self.strk["vs"] = [[Trk() for _ in range(4)] for _ in range(NM)]
        self.wgu_trk = [[Trk() for _ in range(NE)] for _ in range(2)]
        self.t_bd = [Trk() for _ in range(24)]
        self.fin = []

    def build(self):
        nc = self.nc
        with contextlib.ExitStack() as es:
            sems = [es.enter_context(nc.semaphore(f"s{i}")) for i in range(100)]
            self.P = Prog(nc, sems)
            self.banks = [es.enter_context(nc.psum_tensor(f"bank{i}", [128, 512], F32)) for i in range(8)]
            self.bank_trk = [Trk() for _ in range(8)]
            self.bank_rr = 0
            self._uid = 0

            def uname(name):
                self._uid += 1
                return f"sb{self._uid}_{name}"
            sbg = lambda name, shape, dt=F32: es.enter_context(nc.sbuf_tensor(uname(name), shape, dt))
            self.ident = sbg("ident", [128, 128]); self.t_ident = Trk()
            self.ones_f = sbg("ones_f", [128, 128]); self.t_ones_f = Trk()
            self.ones_b = sbg("ones_b", [128, 128], BF16); self.t_ones_b = Trk()
            self.flags = sbg("flags", [128, 2]); self.t_flags = Trk()
            self.adacol = sbg("adacol", [128, 2, 4, KC]); self.t_adacol = Trk()
            self.kvcol = sbg("kvcol", [128, 2, KC]); self.t_kvcol = Trk()
            self.t_gbc = [Trk() for _ in range(4)]
            P = self.P
            P.dma("sync", self.ident[:], self.I["ident"], writes=[self.t_ident])
            P.dma("sync", self.flags[:], self.I["flags"], writes=[self.t_flags])
            P.op("gpsimd", lambda e: e.memset(self.ones_f[:], 1.0), writes=[self.t_ones_f])
            P.op("gpsimd", lambda e: e.memset(self.ones_b[:], 1.0), writes=[self.t_ones_b])
            for ph in self.phases:
                with contextlib.ExitStack() as pes:
                    self.sb = lambda name, shape, dt=F32, pes=pes: pes.enter_context(nc.sbuf_tensor(uname(name), shape, dt))
                    getattr(self, "phase_" + ph)()
                    last = ph == self.phases[-1]
                    P.run_block(self.fin if last else ())
        return nc

    def bank(self):
        i = self.bank_rr
        self.bank_rr = (i + 1) % 8
        return self.banks[i], self.bank_trk[i]

    def mm(self, items, reads, writes):
        def fn(e):
            ins = None
            for (o, l, r, st, sp) in items:
                ins = e.matmul(o, lhsT=l, rhs=r, start=st, stop=sp)
            return ins
        return self.P.op("tensor", fn, reads, writes)

    def mmacc(self, out, pairs, reads, writes):
        n = len(pairs)
        return self.mm([(out, l, r, i == 0, i == n - 1) for i, (l, r) in enumerate(pairs)], reads, writes)

    def tr(self, items, reads, writes):
        ident = self.ident

        def fn(e):
            ins = None
            for (o, i) in items:
                ins = e.transpose(o, i, ident[:])
            return ins
        return self.P.op("tensor", fn, list(reads) + [self.t_ident], writes)

    def act(self, out, in_, func, reads, writes, scale=None, bias=None):
        kw = {}
        if scale is not None:
            kw["scale"] = scale
        if bias is not None:
            kw["bias"] = bias
        return self.P.op("scalar", lambda e: e.activation(out=out, in_=in_, func=func, **kw), reads, writes)

    def tt(self, eng, out, in0, in1, op, reads, writes):
        return self.P.op(eng, lambda e: e.tensor_tensor(out=out, in0=in0, in1=in1, op=op), reads, writes)

    def stt(self, eng, out, in0, scalar, in1, op0, op1, reads, writes):
        return self.P.op(eng, lambda e: e.scalar_tensor_tensor(out=out, in0=in0, scalar=scalar, in1=in1, op0=op0, op1=op1), reads, writes)

    def ts(self, eng, out, in0, s1, s2, op0, op1, reads, writes):
        if s2 is None:
            return self.P.op(eng, lambda e: e.tensor_scalar(out=out, in0=in0, scalar1=s1, scalar2=None, op0=op0), reads, writes)
        return self.P.op(eng, lambda e: e.tensor_scalar(out=out, in0=in0, scalar1=s1, scalar2=s2, op0=op0, op1=op1), reads, writes)

    def cp(self, eng, out, in_, reads, writes):
        if eng == "scalar":
            return self.P.op(eng, lambda e: e.copy(out=out, in_=in_), reads, writes)
        return self.P.op(eng, lambda e: e.tensor_copy(out=out, in_=in_), reads, writes)

    def load_gb(self, gi):
        gb = self.sb("gb", [128, D])
        t = Trk()
        self.P.dma("sync", gb[:], self.S["gbcd"][gi], reads=[self.t_gbc[gi]], writes=[t])
        return gb, t

    def load_xt(self, src, row0, xt, trk, src_trk=()):
        ap = src[row0:row0 + MT, :].rearrange("(t p) d -> p t d", p=128)
        return self.P.dma("sync", xt[:], ap, reads=list(src_trk), writes=list(trk))

    def make_hT(self, xt, xt_trk, hT, hT_trk, sc_ap, sh_ap, col_trk, ntt=4):
        for c in range(KC):
            bk, bt = self.bank()
            self.tr([(bk[:, t * 128:(t + 1) * 128], xt[:, t, c * 128:(c + 1) * 128]) for t in range(ntt)],
                    list(xt_trk)[0:ntt], [bt])
            self.act(hT[:, c, 0:ntt * 128], bk[:, 0:ntt * 128], AF.Identity, [bt, col_trk], [hT_trk],
                     scale=sc_ap[:, c:c + 1], bias=sh_ap[:, c:c + 1])

    def layer_norm(self, zz, trk, lng, lnb, lntrk, st, mv, sttrk):
        P = self.P

        def stats(e):
            e.bn_stats(out=st[:, 0, :], in_=zz[:, 0:512])
            return e.bn_stats(out=st[:, 1, :], in_=zz[:, 512:1024])
        P.op("vector", stats, [trk], [sttrk])
        P.op("vector", lambda e: e.bn_aggr(out=mv[:, 0:2], in_=st[:]), [sttrk], [sttrk])
        self.act(mv[:, 2:3], mv[:, 1:2], AF.Sqrt, [sttrk], [sttrk], scale=1.0, bias=LN_EPS)
        P.op("vector", lambda e: e.reciprocal(out=mv[:, 3:4], in_=mv[:, 2:3]), [sttrk], [sttrk])
        self.ts("vector", zz, zz, mv[:, 0:1], mv[:, 3:4], ALU.subtract, ALU.mult, [trk, sttrk], [trk])
        self.tt("vector", zz, zz, lng, ALU.mult, [trk, lntrk], [trk])
        self.tt("gpsimd", zz, zz, lnb, ALU.add, [trk, lntrk], [trk])

    def phase_setup(self):
        P, I, S = self.P, self.I, self.S
        sb = self.sb
        ccol = sb("ccol", [128, KC]); t_c = Trk()
        crep = sb("crep", [128, KC, 128]); t_crep = Trk()
        stage = [sb(f"adst{i}", [128, KC, 512]) for i in range(2)]; t_stage = [Trk(), Trk()]
        brow = [sb(f"brow{i}", [1, 512]) for i in range(2)]; t_brow = [Trk(), Trk()]
        tmp = [sb(f"adtmp{i}", [128, 512]) for i in range(2)]; t_tmp = [Trk(), Trk()]
        P.dma("sync", ccol[:], I["c_col"], writes=[t_c])
        self.act(ccol[:], ccol[:], AF.Silu, [t_c], [t_c])
        for k in range(KC):
            self.cp("vector", crep[:, k, :], ccol[:, k:k + 1].to_broadcast([128, 128]), [t_c], [t_crep])
        jobs = []
        for l in range(2):
            for j in range(12):
                jobs.append((I["ada_w"][l], I["ada_b"][l:l + 1, :], j, ("ada", l, j // 2, j % 2)))
        for j in range(4):
            jobs.append((I["kv_ada_w"], I["kv_ada_b"], j, ("kv", 0, j // 2, j % 2)))
        for n, (w, b, j, (kind, l, which, half)) in enumerate(jobs):
            s = n % 2
            P.dma("sync", stage[s][:], w[:, j * 512:(j + 1) * 512].rearrange("(k p) n -> p k n", p=128), writes=[t_stage[s]])
            P.dma("sync", brow[s][:], b[:, j * 512:(j + 1) * 512], writes=[t_brow[s]])
            bk, bt = self.bank()
            items = [(bk[:], crep[:, k, :], stage[s][:, k, :], k == 0, False) for k in range(KC)]
            items.append((bk[:], self.ones_f[0:1, :], brow[s][0:1, :], False, True))
            self.mm(items, [t_crep, t_stage[s], t_brow[s], self.t_ones_f], [bt])
            is_g = kind == "ada" and which in (2, 5)
            is_scale = (kind == "ada" and which in (1, 4)) or (kind == "kv" and which == 1)
            if is_g:
                gi = l * 2 + (0 if which == 2 else 1)
                self.act(tmp[s][:], bk[:], AF.Identity, [bt], [t_tmp[s]], scale=1.0, bias=1.0)
                P.dma("gpsimd", S["gbcd"][gi, :, half * 512:(half + 1) * 512], tmp[s][:], reads=[t_tmp[s]],
                      writes=[self.t_gbc[gi]])
            else:
                self.act(tmp[s][:], bk[:], AF.Identity, [bt], [t_tmp[s]], scale=1.0, bias=(1.0 if is_scale else 0.0))
                bk2, bt2 = self.bank()
                self.tr([(bk2[:, i * 128:(i + 1) * 128], tmp[s][:, i * 128:(i + 1) * 128]) for i in range(4)],
                        [t_tmp[s]], [bt2])
                src = bk2[:, 0:512:128]
                if kind == "ada":
                    ci = {0: 0, 1: 1, 3: 2, 4: 3}[which]
                    self.cp("vector", self.adacol[:, l, ci, half * 4:(half + 1) * 4], src, [bt2], [self.t_adacol])
                else:
                    self.cp("vector", self.kvcol[:, which, half * 4:(half + 1) * 4], src, [bt2], [self.t_kvcol])

    def phase_wconv(self):
        P, I, S = self.P, self.I, self.S
        sb = self.sb
        sg = [sb(f"wcg{i}", [128, KC, 256]) for i in range(2)]; t_sg = [Trk(), Trk()]
        su = [sb(f"wcu{i}", [128, KC, 256]) for i in range(2)]; t_su = [Trk(), Trk()]
        wb = [sb(f"wcb{i}", [128, KC, 512], BF16) for i in range(2)]; t_wb = [Trk(), Trk()]
        n = 0
        for l in range(2):
            for e in range(NE):
                s = n % 2
                n += 1
                P.dma("sync", sg[s][:], I["moe_wg"][l, e].rearrange("(k p) n -> p k n", p=128), writes=[t_sg[s]])
                P.dma("sync", su[s][:], I["moe_wu"][l, e].rearrange("(k p) n -> p k n", p=128), writes=[t_su[s]])
                self.cp("gpsimd", wb[s][:, :, 0:256], sg[s][:], [t_sg[s]], [t_wb[s]])
                self.cp("vector", wb[s][:, :, 256:512], su[s][:], [t_su[s]], [t_wb[s]])
                P.dma("gpsimd", S["wgu"][l, e], wb[s][:], reads=[t_wb[s]], writes=[self.wgu_trk[l][e]])

    def bg_jobs(self):
        I, S = self.I, self.S
        jobs = []
        kp = lambda ap: ap.rearrange("(k p) n -> p k n", p=128)

        def moe_layer(l):
            for e in range(NE):
                jobs.append((S["wgu"][l, e][:, :, 0:256], kp(I["moe_wg"][l, e]), self.wgu_trk[l][e]))
                jobs.append((S["wgu"][l, e][:, :, 256:512], kp(I["moe_wu"][l, e]), self.wgu_trk2[l][e]))
            for e in range(NE):
                jobs.append((S["wdb"][l][:, e, :, :], I["moe_wd"][l, e].rearrange("(f p) n -> p f n", p=128), self.t_wdb[l][e]))
        moe_layer(0)
        jobs.append((S["rwb"], kp(I["router_w"]), self.t_rwb))
        for k in range(KC):
            for hf in range(2):
                jobs.append((S["wkvb"][:, k, hf * 3 * D:(hf + 1) * 3 * D], I["w_kv"][k * 128:(k + 1) * 128, hf * 3 * D:(hf + 1) * 3 * D],
                             self.t_wkvb[2 * k + hf]))
        for k in range(KC):
            jobs.append((S["wqb"][:, k, :], I["w_q"][k * 128:(k + 1) * 128, :], self.t_wqb[k]))
        moe_layer(1)
        return jobs

    def phase_conv(self):
        P, I, S = self.P, self.I, self.S
        sb = self.sb
        win = sb("win", [128, KC, 3 * D], BF16); t_win = [Trk() for _ in range(2 * KC)]
        wout = sb("wout", [128, KC, D], BF16); t_wout = [Trk() for _ in range(KC)]
        wst = [sb(f"wst{i}", [128, 3 * D]) for i in range(2)]; t_wst = [Trk(), Trk()]
        cw = sb("cw", [128, KC, 3]); t_cw = Trk()
        lng = sb("lng", [128, D]); lnb = sb("lnb", [128, D]); t_ln = Trk()
        xt = [sb(f"xt{i}", [128, 4, D]) for i in range(2)]; t_xt = [[Trk() for _ in range(4)] for _ in range(2)]
        hT = [sb(f"hT{i}", [128, KC, MT], BF16) for i in range(2)]; t_hT = [Trk(), Trk()]
        zT = [sb(f"zT{i}", [128, KC, MT], BF16) for i in range(2)]; t_zT = [Trk(), Trk()]
        ub = sb("ub", [128, KC, MT + 2]); t_ub = [Trk() for _ in range(KC)]
        vs = [sb(f"vsb{i}", [128, MT]) for i in range(2)]; t_vs = [Trk(), Trk()]
        acc = [sb(f"acc{i}", [128, MT]) for i in range(2)]; t_acc = [Trk(), Trk()]
        st = sb("st", [128, 2, 6]); mv = sb("mv", [128, 4]); t_st = Trk()
        gb, t_gb = self.load_gb(0)
        P.dma("sync", cw[:], I["conv_wc"], writes=[t_cw])
        P.dma("sync", lng[:], I["ln_g"][0], writes=[t_ln])
        P.dma("sync", lnb[:], I["ln_b"][0], writes=[t_ln])
        for k in range(KC):
            s = k % 2
            P.dma("sync", wst[s][:], I["conv_w_in"][k * 128:(k + 1) * 128, :], writes=[t_wst[s]])
            self.cp("gpsimd", win[:, k, 0:1536], wst[s][:, 0:1536], [t_wst[s]], [t_win[2 * k]])
            self.cp("vector", win[:, k, 1536:3072], wst[s][:, 1536:3072], [t_wst[s]], [t_win[2 * k + 1]])
        for k in range(KC):
            s = k % 2
            P.dma("sync", wst[s][:, 0:D], I["conv_w_out"][k * 128:(k + 1) * 128, :], writes=[t_wst[s]])
            self.tt("vector" if k % 2 == 0 else "gpsimd", wout[:, k, :], wst[s][:, 0:D], gb[:], ALU.mult, [t_wst[s], t_gb], [t_wout[k]])
        sc = self.adacol[:, 0, 1, :]
        sh = self.adacol[:, 0, 0, :]
        hv = self.flags[:, 0:1]

        def proj(hTb, t_h, j, which, n):
            bk, bt = self.bank()
            col = which * D + j * 128
            self.mmacc(bk[:, 0:n], [(win[:, k, col:col + 128], hTb[:, k, 0:n]) for k in range(KC)], t_win + [t_h], [bt])
            return bk, bt

        P.dma("sync", xt[0][:, 0, :], I["xc"], writes=[t_xt[0][0]])
        self.make_hT(xt[0], t_xt[0], hT[0], t_hT[0], sc, sh, self.t_adacol, ntt=1)
        for j in range(KC):
            bc, tc = proj(hT[0], t_hT[0], j, 1, 128)
            bv, tv = proj(hT[0], t_hT[0], j, 2, 128)
            self.cp("scalar", vs[0][:, 0:128], bv[:, 0:128], [tv], [t_vs[0]])
            self.tt("vector", ub[:, j, 0:2], bc[:, 126:128], vs[0][:, 126:128], ALU.mult, [tc, t_vs[0]], [t_ub[j]])

        bgq = self.bg_jobs()
        per_tile = -(-len(bgq) // (NM - 1))
        self.load_xt(I["xin"], 0, xt[0], t_xt[0])
        self.make_hT(xt[0], t_xt[0], hT[0], t_hT[0], sc, sh, self.t_adacol)
        for m in range(NM):
            s = m % 2
            if m + 1 < NM:
                self.load_xt(I["xin"], (m + 1) * MT, xt[1 - s], t_xt[1 - s])
            for _ in range(per_tile):
                if bgq:
                    o_, i_, t_ = bgq.pop(0)
                    P.dma_bg(o_, i_, writes=[t_])
            for j in range(KC):
                b = j % 2
                bB, tB = proj(hT[s], t_hT[s], j, 0, MT)
                bC, tC = proj(hT[s], t_hT[s], j, 1, MT)
                bV, tV = proj(hT[s], t_hT[s], j, 2, MT)
                self.cp("scalar", vs[b][:], bV[:], [tV], [t_vs[b]])
                self.tt("vector", ub[:, j, 2:MT + 2], bC[:], vs[b][:], ALU.mult, [tC, t_vs[b]], [t_ub[j]])
                self.act(acc[b][:], ub[:, j, 0:MT], AF.Copy, [t_ub[j], t_cw], [t_acc[b]], scale=cw[:, j, 0:1])
                self.stt("vector", acc[b][:], ub[:, j, 1:MT + 1], cw[:, j, 1:2], acc[b][:], ALU.mult, ALU.add,
                         [t_ub[j], t_cw, t_acc[b]], [t_acc[b]])
                self.stt("vector", acc[b][:], ub[:, j, 2:MT + 2], cw[:, j, 2:3], acc[b][:], ALU.mult, ALU.add,
                         [t_ub[j], t_cw, t_acc[b]], [t_acc[b]])
                self.tt("vector", zT[s][:, j, :], bB[:], acc[b][:], ALU.mult, [tB, t_acc[b]], [t_zT[s]])
                if m == NH - 1:
                    self.ts("gpsimd", ub[:, j, 0:2], ub[:, j, MT:MT + 2], hv, None, ALU.mult, None,
                            [t_ub[j], self.t_flags], [t_ub[j]])
                else:
                    self.cp("gpsimd", ub[:, j, 0:2], ub[:, j, MT:MT + 2], [t_ub[j]], [t_ub[j]])
            if m + 1 < NM:
                self.make_hT(xt[1 - s], t_xt[1 - s], hT[1 - s], t_hT[1 - s], sc, sh, self.t_adacol)
            for t in range(4):
                for half in range(2):
                    bk, bt = self.bank()
                    self.mmacc(bk[:], [(zT[s][:, k, t * 128:(t + 1) * 128], wout[:, k, half * 512:(half + 1) * 512])
                                       for k in range(KC)], [t_zT[s]] + t_wout, [bt])
                    zz = xt[s][:, t, half * 512:(half + 1) * 512]
                    self.stt("vector", zz, zz, ALPHA, bk[:], ALU.mult, ALU.add, [t_xt[s][t], bt], [t_xt[s][t]])
                self.layer_norm(xt[s][:, t, :], t_xt[s][t], lng[:], lnb[:], t_ln, st, mv, t_st)
            P.dma("gpsimd", S["x1s"][m * MT:(m + 1) * MT, :].rearrange("(t p) d -> p t d", p=128), xt[s][:],
                  reads=t_xt[s], writes=[self.strk["x1s"][m]])

    def phase_moe0(self):
        self.moe(0, self.S["x1s"], self.strk["x1s"], self.S["x2s"], self.strk["x2s"], range(NM), 0)

    def phase_moe1(self):
        self.moe(1, self.S["x3s"], self.strk["x3s"], self.out, None, range(NO), 0)

    def moe(self, l, src, src_trk, dst, dst_trk, tiles, row_off):
        P, I, S = self.P, self.I, self.S
        sb = self.sb
        nc = self.nc
        wd = sb("wd", [128, NE, 2, D], BF16); t_wd = [Trk() for _ in range(4)]
        wgu = [sb(f"wgu{i}", [128, KC, 512], BF16) for i in range(3)]; t_wgu = [Trk() for _ in range(3)]
        rw = sb("rw", [128, KC, NE], BF16); t_rw = Trk()
        rb = sb("rb", [128, NE]); t_rb = Trk()
        lng = sb("lng", [128, D]); lnb = sb("lnb", [128, D]); t_ln = Trk()
        xt = [sb(f"xt{i}", [128, 4, D]) for i in range(2)]; t_xt = [[Trk() for _ in range(4)] for _ in range(2)]
        he = sb("he", [128, NE, 2, MT], BF16); t_he = [Trk() for _ in range(NE)]
        sgb = [sb(f"sgb{i}", [128, MT]) for i in range(2)]; t_sgb = [Trk(), Trk()]
        tb = [sb(f"tb{i}", [128, MT]) for i in range(2)]; t_tb = [Trk(), Trk()]
        cb = [sb(f"cb{i}", [128, MT]) for i in range(4)]; t_cb = [Trk() for _ in range(4)]
        cTf = [sb(f"cTf{i}", [NE, MT]) for i in range(2)]; t_cTf = [Trk(), Trk()]
        t_cbd = [Trk(), Trk()]
        st = sb("st", [128, 2, 6]); mv = sb("mv", [128, 4]); t_st = Trk()
        aff = sb("aff", [128, 4, NE]); selv = sb("selv", [128, 4, NE]); msk = sb("msk", [128, 4, NE])
        eq = sb("eq", [128, 4, NE]); m1 = sb("m1", [128, 4, 4]); m2 = sb("m2", [128, 4, 4]); gs = sb("gs", [128, 4, 4])
        gm = sb("gm", [128, 4]); t1 = sb("t1", [128, 4]); comb = sb("comb", [128, 4, NE])
        hT0 = sb("hT0", [128, KC, MT], BF16)
        t_r = Trk()
        li = 2 * l + 1
        P.dma("sync", lng[:], I["ln_g"][li], writes=[t_ln])
        P.dma("sync", lnb[:], I["ln_b"][li], writes=[t_ln])
        P.dma("sync", rb[:], I["router_b"], writes=[t_rb])
        gi = l * 2 + 1
        gb = sb("gb", [128, D]); t_gb = Trk()
        P.dma("sync", gb[:], S["gbcd"][gi], reads=[self.t_gbc[gi]], writes=[t_gb])
        P.dma("sync", rw[:], S["rwb"], reads=[self.t_rwb], writes=[t_rw])
        for e4 in range(4):
            P.dma("sync", wd[:, e4 * 4:(e4 + 1) * 4, :, :], S["wdb"][l][:, e4 * 4:(e4 + 1) * 4, :, :],
                  reads=self.t_wdb[l][e4 * 4:(e4 + 1) * 4], writes=[t_wd[e4]])
        hT1 = sb("hT1", [128, KC, MT], BF16)
        gtmp = [sb(f"gtmp{i}", [128, MT]) for i in range(1)]; t_gtmp = [Trk()]
        hT = [hT0, hT1]; t_hT = [Trk(), Trk()]
        sc = self.adacol[:, l, 3, :]
        sh = self.adacol[:, l, 2, :]
        V = "vector"
        n_wgu = 0
        tiles = list(tiles)
        nt = len(tiles)
        pending = []

        def fetch(e):
            nonlocal n_wgu
            s = n_wgu % 3
            n_wgu += 1
            P.dma("sync", wgu[s][:], S["wgu"][l, e], reads=[self.wgu_trk[l][e], self.wgu_trk2[l][e]], writes=[t_wgu[s]])
            pending.append(s)

        def rmax(out, in_):
            return P.op(V, lambda e: e.tensor_reduce(out=out, in_=in_, axis=mybir.AxisListType.X, op=ALU.max), [t_r], [t_r])

        def front_a(mi):
            s = mi % 2
            hTb, thT = hT[s], t_hT[s]
            self.make_hT(xt[s], t_xt[s], hTb, thT, sc, sh, self.t_adacol)
            bk, bt = self.bank()
            items = []
            for t in range(4):
                for k in range(KC):
                    items.append((bk[:, t * NE:(t + 1) * NE], hTb[:, k, t * 128:(t + 1) * 128], rw[:, k, :], k == 0, k == KC - 1))
            self.mm(items, [thT, t_rw], [bt])
            lg = bk[:, 0:4 * NE].rearrange("p (t e) -> p t e", t=4)
            self.act(aff[:], lg, AF.Sigmoid, [bt], [t_r])

        def chain():
            th = []
            A = th.append
            A(lambda: self.tt(V, selv[:], aff[:], rb[:].unsqueeze(1).to_broadcast([128, 4, NE]), ALU.add, [t_r, t_rb], [t_r]))
            sel4 = selv[:].rearrange("p t (g i) -> p (t g) i", g=4)
            eq4 = eq[:].rearrange("p t (g i) -> p (t g) i", g=4)
            m1f = m1[:].rearrange("p t g -> p (t g)")
            m2f = m2[:].rearrange("p t g -> p (t g)")
            A(lambda: rmax(m1f, sel4))
            A(lambda: self.tt(V, eq4, sel4, m1f.unsqueeze(2).to_broadcast([128, 16, 4]), ALU.is_equal, [t_r], [t_r]))
            A(lambda: self.stt(V, eq4, eq4, NEG, sel4, ALU.mult, ALU.add, [t_r], [t_r]))
            A(lambda: rmax(m2f, eq4))
            A(lambda: self.tt(V, gs[:], m1[:], m2[:], ALU.add, [t_r], [t_r]))
            A(lambda: rmax(gm[:], gs[:]))
            A(lambda: self.tt(V, gs[:], gs[:], gm[:].unsqueeze(2).to_broadcast([128, 4, 4]), ALU.is_equal, [t_r], [t_r]))
            A(lambda: self.ts(V, gs[:], gs[:], -1.0, -NEG, ALU.add, ALU.mult, [t_r], [t_r]))
            gsf = gs[:].rearrange("p t g -> p (t g)")
            msk4 = msk[:].rearrange("p t (g i) -> p (t g) i", g=4)
            A(lambda: self.tt(V, msk4, sel4, gsf.unsqueeze(2).to_broadcast([128, 16, 4]), ALU.add, [t_r], [t_r]))
            A(lambda: rmax(t1[:], msk[:]))
            A(lambda: self.tt(V, eq[:], msk[:], t1[:].unsqueeze(2).to_broadcast([128, 4, NE]), ALU.is_equal, [t_r], [t_r]))
            A(lambda: self.stt(V, msk[:], eq[:], NEG, msk[:], ALU.mult, ALU.add, [t_r], [t_r]))
            A(lambda: rmax(t1[:], msk[:]))
            A(lambda: self.tt(V, selv[:], msk[:], t1[:].unsqueeze(2).to_broadcast([128, 4, NE]), ALU.is_equal, [t_r], [t_r]))
            A(lambda: self.tt(V, eq[:], eq[:], selv[:], ALU.add, [t_r], [t_r]))
            A(lambda: self.tt(V, comb[:], aff[:], eq[:], ALU.mult, [t_r], [t_r]))
            A(lambda: P.op(V, lambda e: e.tensor_reduce(out=t1[:], in_=comb[:], axis=mybir.AxisListType.X, op=ALU.add), [t_r], [t_r]))
            A(lambda: P.op(V, lambda e: e.reciprocal(out=t1[:], in_=t1[:]), [t_r], [t_r]))
            A(lambda: self.tt(V, comb[:], comb[:], t1[:].unsqueeze(2).to_broadcast([128, 4, NE]), ALU.mult, [t_r], [t_r]))
            return th

        def front_c(mi):
            s = mi % 2
            bk, bt = self.bank()
            self.tr([(bk[0:NE, t * 128:(t + 1) * 128], comb[:, t, :]) for t in range(4)], [t_r], [bt])
            self.cp("scalar", cTf[s][:], bk[0:NE, :], [bt], [t_cTf[s]])
            P.dma("sync", S["cbd"][l, s], cTf[s][:], reads=[t_cTf[s]], writes=[t_cbd[s]])

        def fetch_cb(mi, e):
            s = mi % 2
            b = e % 4
            P.dma("sync", cb[b][:], S["cbd"][l, s][e:e + 1, :].partition_broadcast(128), reads=[t_cbd[s]], writes=[t_cb[b]])

        def expert(mi, e):
            s = mi % 2
            hTb, thT = hT[s], t_hT[s]
            if True:
                ws = pending.pop(0)
                nxt = e + 2
                if nxt < NE or mi + 1 < nt:
                    fetch(nxt % NE)
                b = e % 4
                if e + 2 < NE:
                    fetch_cb(mi, e + 2)
                for f in range(2):
                    bb = f
                    bkg, btg = self.bank()
                    self.mmacc(bkg[:], [(wgu[ws][:, k, f * 128:(f + 1) * 128], hTb[:, k, :]) for k in range(KC)],
                               [t_wgu[ws], thT], [btg])
                    bku, btu = self.bank()
                    self.mmacc(bku[:], [(wgu[ws][:, k, 256 + f * 128:256 + (f + 1) * 128], hTb[:, k, :]) for k in range(KC)],
                               [t_wgu[ws], thT], [btu])
                    self.act(sgb[bb][:], bkg[:], AF.Silu, [btg], [t_sgb[bb]])
                    self.tt(V, tb[bb][:], bku[:], cb[b][:], ALU.mult, [btu, t_cb[b]], [t_tb[bb]])
                    self.tt("gpsimd", he[:, e, f, :], sgb[bb][:], tb[bb][:], ALU.mult, [t_sgb[bb], t_tb[bb]], [t_he[e]])

        def back(mi):
            s = mi % 2
            m = tiles[mi]
            for t in range(4):
                for half in range(2):
                    bk, bt = self.bank()
                    pairs = []
                    for e in range(NE):
                        for f in range(2):
                            pairs.append((he[:, e, f, t * 128:(t + 1) * 128], wd[:, e, f, half * 512:(half + 1) * 512]))
                    self.mmacc(bk[:], pairs, t_he + t_wd, [bt])
                    zz = xt[s][:, t, half * 512:(half + 1) * 512]
                    gq = 0
                    self.tt(V, gtmp[gq][:], bk[:], gb[:, half * 512:(half + 1) * 512], ALU.mult, [bt, t_gb], [t_gtmp[gq]])
                    self.stt(V, zz, zz, ALPHA, gtmp[gq][:], ALU.mult, ALU.add, [t_xt[s][t], t_gtmp[gq]], [t_xt[s][t]])
                self.layer_norm(xt[s][:, t, :], t_xt[s][t], lng[:], lnb[:], t_ln, st, mv, t_st)
            tok = P.dma("gpsimd", dst[m * MT:(m + 1) * MT, :].rearrange("(t p) d -> p t d", p=128), xt[s][:],
                        reads=t_xt[s], writes=([dst_trk[m]] if dst_trk is not None else []))
            if dst_trk is None:
                self.fin.append(tok)

        def load(mi):
            m = tiles[mi]
            self.load_xt(src, m * MT, xt[mi % 2], t_xt[mi % 2], [src_trk[m]])

        load(0)
        if nt > 1:
            load(1)
        fetch(0)
        fetch(1)
        front_a(0)
        for th in chain():
            th()
        front_c(0)
        for mi in range(nt):
            cq = []
            fetch_cb(mi, 0)
            fetch_cb(mi, 1)
            for e in range(NE):
                if e == 5 and mi + 1 < nt:
                    front_a(mi + 1)
                    cq = chain()
                expert(mi, e)
                for _ in range(4):
                    if cq:
                        cq.pop(0)()
            while cq:
                cq.pop(0)()
            if mi + 1 < nt:
                front_c(mi + 1)
            back(mi)
            if mi + 2 < nt:
                load(mi + 2)

    def phase_kv(self):
        P, I, S = self.P, self.I, self.S
        sb = self.sb
        w = sb("wkv", [128, KC, 6 * D], BF16); t_wl = [Trk() for _ in range(KC)]
        xt = [sb(f"xt{i}", [128, 4, D]) for i in range(2)]; t_xt = [[Trk() for _ in range(4)] for _ in range(2)]
        hT = [sb(f"hT{i}", [128, KC, MT], BF16) for i in range(2)]; t_hT = [Trk(), Trk()]
        kst = [sb(f"kst{i}", [128, 8, MT], BF16) for i in range(3)]; t_kst = [[Trk() for _ in range(8)] for _ in range(3)]
        vst = [sb(f"vst{i}", [128, 3 * D], BF16) for i in range(2)]; t_vst = [[Trk() for _ in range(6)] for _ in range(2)]
        for k in range(KC):
            P.dma("sync", w[:, k, :], S["wkvb"][:, k, :], reads=self.t_wkvb[2 * k:2 * k + 2], writes=[t_wl[k]])
        sc = self.kvcol[:, 1, :]
        sh = self.kvcol[:, 0, :]
        ne = 0
        nk = 0
        nv = 0
        self.load_xt(S["x2s"], 0, xt[0], t_xt[0], [self.strk["x2s"][0]])
        self.make_hT(xt[0], t_xt[0], hT[0], t_hT[0], sc, sh, self.t_kvcol)
        for m in range(NM):
            s = m % 2
            if m + 1 < NM:
                self.load_xt(S["x2s"], (m + 1) * MT, xt[1 - s], t_xt[1 - s], [self.strk["x2s"][m + 1]])
            g2_only = m < NH - 1
            for c8 in ([2] if g2_only else range(3)):
                ks = nk % 3
                nk += 1
                for c in range(8):
                    ch = c8 * 8 + c
                    bk, bt = self.bank()
                    self.mmacc(bk[:], [(w[:, k, ch * 128:(ch + 1) * 128], hT[s][:, k, :]) for k in range(KC)], t_wl + [t_hT[s]], [bt])
                    self.cp("scalar" if ne % 2 == 0 else "vector", kst[ks][:, c, :], bk[:], [bt], [t_kst[ks][c]])
                    ne += 1
                P.dma("gpsimd", S["kts"][c8 * 8:(c8 + 1) * 8, :, m * MT:(m + 1) * MT].rearrange("c p t -> p c t"), kst[ks][:],
                      reads=t_kst[ks], writes=[self.strk["kts"][m][c8]])
            if m + 1 < NM:
                self.make_hT(xt[1 - s], t_xt[1 - s], hT[1 - s], t_hT[1 - s], sc, sh, self.t_kvcol)
            for t in range(4):
                vi = nv % 2
                nv += 1
                cbs = [4, 5] if g2_only else list(range(6))
                for cbk in cbs:
                    bk, bt = self.bank()
                    self.mmacc(bk[:], [(hT[s][:, k, t * 128:(t + 1) * 128], w[:, k, 3 * D + cbk * 512:3 * D + (cbk + 1) * 512])
                                       for k in range(KC)], t_wl + [t_hT[s]], [bt])
                    self.cp("scalar" if ne % 2 == 0 else "vector", vst[vi][:, cbk * 512:(cbk + 1) * 512], bk[:], [bt], [t_vst[vi][cbk]])
                    ne += 1
                c0 = cbs[0] * 512
                P.dma("gpsimd", S["vs"][m * MT + t * 128:m * MT + (t + 1) * 128, c0:3 * D], vst[vi][:, c0:3 * D],
                      reads=t_vst[vi], writes=[self.strk["vs"][m][t]])

    def phase_q(self):
        P, I, S = self.P, self.I, self.S
        sb = self.sb
        w = sb("wq", [128, KC, 3 * D], BF16); t_wl = [Trk() for _ in range(KC)]
        xt = [sb(f"xt{i}", [128, 4, D]) for i in range(2)]; t_xt = [[Trk() for _ in range(4)] for _ in range(2)]
        hT = [sb(f"hT{i}", [128, KC, MT], BF16) for i in range(2)]; t_hT = [Trk(), Trk()]
        qst = sb("qst", [128, 24, MT], BF16); t_qst = [Trk() for _ in range(24)]
        qs = 128.0 ** -0.5
        self.bias_tables_a()
        for k in range(KC):
            P.dma("sync", w[:, k, :], S["wqb"][:, k, :], reads=[self.t_wqb[k]], writes=[t_wl[k]])
        qcol = sb("qcol", [128, 2, KC]); t_qcol = Trk()
        self.ts("vector", qcol[:, 0, :], self.adacol[:, 1, 1, :], qs, None, ALU.mult, None, [self.t_adacol], [t_qcol])
        self.ts("vector", qcol[:, 1, :], self.adacol[:, 1, 0, :], qs, None, ALU.mult, None, [self.t_adacol], [t_qcol])
        sc = qcol[:, 0, :]
        sh = qcol[:, 1, :]
        ne = 0
        self.load_xt(S["x2s"], NH * MT, xt[0], t_xt[0], [self.strk["x2s"][NH]])
        self.make_hT(xt[0], t_xt[0], hT[0], t_hT[0], sc, sh, t_qcol)
        for mo in range(NO):
            m = NH + mo
            s = mo % 2
            if mo == 4:
                self.bias_tables_b()
            if mo + 1 < NO:
                self.load_xt(S["x2s"], (m + 1) * MT, xt[1 - s], t_xt[1 - s], [self.strk["x2s"][m + 1]])
            for ch in range(24):
                if ch == 12 and mo + 1 < NO:
                    self.make_hT(xt[1 - s], t_xt[1 - s], hT[1 - s], t_hT[1 - s], sc, sh, t_qcol)
                bk, bt = self.bank()
                self.mmacc(bk[:], [(w[:, k, ch * 128:(ch + 1) * 128], hT[s][:, k, :]) for k in range(KC)], t_wl + [t_hT[s]], [bt])
                self.cp("scalar" if ne % 2 == 0 else "vector", qst[:, ch, :], bk[:], [bt], [t_qst[ch]])
                ne += 1
            for c8 in range(3):
                P.dma("gpsimd", S["qts"][c8 * 8:(c8 + 1) * 8, :, mo * MT:(mo + 1) * MT].rearrange("c p t -> p c t"),
                      qst[:, c8 * 8:(c8 + 1) * 8, :], reads=t_qst[c8 * 8:(c8 + 1) * 8], writes=[self.strk["qts"][mo][c8]])

    def bias_tables_a(self):
        P, I, S = self.P, self.I, self.S
        sb = self.sb
        B = self._bt = {}
        B["anti"] = sb("anti", [128, 128]); B["t_anti"] = Trk()
        rel = sb("rel", [32, 24]); oh = sb("oh", [32, 3, 384]); mrow = sb("mrow", [1, 384]); t_rel = Trk()
        fsb = [sb(f"fsb{i}", [8, 384]) for i in range(3)]; t_fsb = [Trk() for _ in range(3)]
        B["hk"] = [sb(f"hk{i}", [128, 256]) for i in range(24)]; B["t_hk"] = [Trk() for _ in range(24)]
        B["bsb"] = [sb(f"bsb{i}", [128, 256]) for i in range(4)]; B["t_bsb"] = [Trk() for _ in range(4)]
        P.dma("gpsimd", B["anti"][:], I["antiid"], writes=[B["t_anti"]])
        P.dma("gpsimd", rel[:], I["rel_bias"], writes=[t_rel])
        P.dma("gpsimd", oh[:], I["oh"].rearrange("g b i -> b g i"), writes=[t_rel])
        P.dma("gpsimd", mrow[:], I["mask"], writes=[t_rel])
        t_fd = [Trk() for _ in range(3)]
        for g in range(3):
            bk, bt = self.bank()
            self.mm([(bk[0:8, 0:384], rel[:, g * 8:(g + 1) * 8], oh[:, g, :], True, False),
                     (bk[0:8, 0:384], self.ones_f[0:1, 0:8], mrow[0:1, :], False, True)], [t_rel, self.t_ones_f], [bt])
            self.cp("vector", fsb[g][:], bk[0:8, 0:384], [bt], [t_fsb[g]])
            P.dma("gpsimd", S["fd"][g], fsb[g][:], reads=[t_fsb[g]], writes=[t_fd[g]])
        fd_t = self.H["fd"]
        for ch in range(24):
            g, h = ch // 8, ch % 8
            src = bass.AP(fd_t, (g * 8 + h) * 384, [[1, 128], [128, 2], [1, 128]])
            P.dma("gpsimd", B["hk"][ch][:].rearrange("p (c q) -> p c q", c=2), src, reads=[t_fd[g]], writes=[B["t_hk"][ch]])

    def bias_tables_b(self):
        P, S = self.P, self.S
        B = self._bt
        for ch in range(24):
            s = ch % 4
            bk, bt = self.bank()
            self.mm([(bk[:, 0:256], B["anti"][:], B["hk"][ch][:], True, True)], [B["t_anti"], B["t_hk"][ch]], [bt])
            self.cp("vector", B["bsb"][s][:], bk[:, 0:256], [bt], [B["t_bsb"][s]])
            P.dma("gpsimd", S["biasd"][ch], B["bsb"][s][:], reads=[B["t_bsb"][s]], writes=[self.t_bd[ch]])

    def phase_attn(self):
        P, I, S = self.P, self.I, self.S
        sb = self.sb
        nc = self.nc
        wo = sb("wo", [128, KC, D], BF16); t_wo = [Trk() for _ in range(KC)]
        wst = [sb(f"wst{i}", [128, D]) for i in range(2)]; t_wst = [Trk(), Trk()]
        lng = sb("lng", [128, D]); lnb = sb("lnb", [128, D]); t_ln = Trk()
        qt = [sb(f"qt{i}", [128, 2048], BF16) for i in range(2)]; t_qt = [Trk(), Trk()]
        kt = [sb(f"kt{i}", [128, 4096], BF16) for i in range(2)]; t_kt = [Trk(), Trk()]
        vt = [sb(f"vt{i}", [128, 32, 128], BF16) for i in range(2)]; t_vt = [[Trk() for _ in range(4)] for _ in range(2)]
        bj = [sb(f"bj{i}", [128, 256]) for i in range(2)]; t_bj = [Trk(), Trk()]
        bjf = [sb(f"bjf{i}", [128, 256]) for i in range(2)]
        acc = [sb(f"acc{i}", [128, 2, 2048]) for i in range(2)]; t_accj = [[Trk()], [Trk()]]
        rcp = sb("rcp", [128, 2048]); t_rcp = Trk()
        mix = sb("mix", [128, KC, 2048], BF16); t_mix = Trk()
        tsb = [sb(f"tsb{i}", [128, 256]) for i in range(6)]; t_tsb = [Trk() for _ in range(6)]
        pT = [sb(f"pT{i}", [128, 256], BF16) for i in range(6)]; t_pT = [Trk() for _ in range(6)]
        xt = [sb(f"xt{i}", [128, 4, D]) for i in range(1)]; t_xt = [[Trk() for _ in range(4)]]
        st = sb("st", [128, 2, 6]); mv = sb("mv", [128, 4]); t_st = Trk()
        hm = self.flags[:, 1:2]
        gb, t_gb = self.load_gb(2)
        P.dma("sync", lng[:], I["ln_g"][2], writes=[t_ln])
        P.dma("sync", lnb[:], I["ln_b"][2], writes=[t_ln])
        for k in range(KC):
            s = k % 2
            P.dma("sync", wst[s][:], I["w_o"][k * 128:(k + 1) * 128, :], writes=[t_wst[s]])
            self.tt("vector" if k % 2 == 0 else "gpsimd", wo[:, k, :], wst[s][:], gb[:], ALU.mult, [t_wst[s], t_gb], [t_wo[k]])
        t_bd = self.t_bd
        LAG = 4
        jobs = [(Ssup, h, g) for Ssup in range(2) for h in range(KC) for g in range(3)]

        def job_dmas(k):
            Ssup, h, g = jobs[k]
            js = k % 2
            r = DIL[g]
            W = 128 * r
            ch = g * 8 + h
            T0 = NH * MT + Ssup * 2048
            span = W + 2048
            nchunk = span // W
            m_lo = (T0 - W) // MT
            m_hi = (T0 + 2048 - 1) // MT
            P.dma("sync", qt[js][:], S["qts"][ch, :, Ssup * 2048:(Ssup + 1) * 2048],
                  reads=[self.strk["qts"][Ssup * 4 + i][g] for i in range(4)], writes=[t_qt[js]])
            P.dma("sync", kt[js][:, 0:span], S["kts"][ch, :, T0 - W:T0 + 2048],
                  reads=[self.strk["kts"][m][g] for m in range(m_lo, m_hi + 1)], writes=[t_kt[js]])
            vs_t = self.H["vs"]
            vs_reads = [tk for m in range(m_lo, m_hi + 1) for tk in self.strk["vs"][m]]
            if r == 1:
                for c0 in range(0, nchunk, 8):
                    c1 = min(nchunk, c0 + 8)
                    src = bass.AP(vs_t, (T0 - W + 128 * c0) * 3 * D + ch * 128, [[3 * D, 128], [128 * 3 * D, c1 - c0], [1, 128]])
                    P.dma("sync", vt[js][:, c0:c1, :], src, reads=vs_reads, writes=[t_vt[js][c0 // 8]])
            else:
                step = 8 if r == 16 else 4
                part = 0
                for c in range(nchunk):
                    for rho0 in range(0, r, step):
                        src = bass.AP(vs_t, (T0 - W + rho0 + 128 * r * c) * 3 * D + ch * 128,
                                      [[r * 3 * D, 128], [3 * D, step], [1, 128]])
                        a0 = rho0 * nchunk + c
                        dstv = vt[js][:, a0:a0 + (step - 1) * nchunk + 1:nchunk, :]
                        P.dma("sync", dstv, src, reads=vs_reads, writes=[t_vt[js][part % 4]])
                        part += 1
            P.dma("sync", bj[js][:], S["biasd"][ch], reads=[t_bd[ch]], writes=[t_bj[js]])
            if Ssup == 0:
                self.cp("gpsimd", bjf[js][:, 0:128], bj[js][:, 0:128], [t_bj[js]], [t_bj[js]])
                self.ts("gpsimd", bjf[js][:, 128:256], bj[js][:, 128:256], hm, None, ALU.add, None,
                        [t_bj[js], self.t_flags], [t_bj[js]])

        pend = []
        nu = 0
        self._nev = 0
        olsb = [sb(f"olsb{i}", [128, 256]) for i in range(6)]; t_ol = [Trk() for _ in range(6)]

        def stage2(js, u4, vcur, vprv, a_out, first, t_prev, t_a):
            bkO, btO = self.bank()
            self.mm([(bkO[:, 0:128], vcur, pT[u4][:, 0:128], True, False),
                     (bkO[:, 0:128], vprv, pT[u4][:, 128:256], False, True),
                     (bkO[:, 128:256], self.ones_b[:], pT[u4][:, 0:128], True, False),
                     (bkO[:, 128:256], self.ones_b[:], pT[u4][:, 128:256], False, True)],
                    t_vt[js] + [t_pT[u4], self.t_ones_b], [btO])
            src = bkO[:, 0:256].rearrange("p (a q) -> p a q", a=2)
            ev = "scalar" if self._nev % 2 == 0 else "vector"
            self._nev += 1
            ol3 = olsb[u4][:].rearrange("p (a q) -> p a q", a=2)
            if first:
                if ev == "scalar":
                    P.op(ev, lambda e: e.copy(out=a_out, in_=src), [btO], [t_a], after=t_prev)
                else:
                    P.op(ev, lambda e: e.tensor_copy(out=a_out, in_=src), [btO], [t_a], after=t_prev)
            elif self._nev % 3 == 0:
                P.op("vector", lambda e: e.tensor_tensor(out=a_out, in0=a_out, in1=src, op=ALU.add), [btO], [t_a], after=t_prev)
            else:
                self.cp("scalar", ol3, src, [btO], [t_ol[u4]])
                P.op("gpsimd", lambda e: e.tensor_tensor(out=a_out, in0=a_out, in1=ol3, op=ALU.add), [t_ol[u4]], [t_a], after=t_prev)

        job_dmas(0)
        for k, (Ssup, h, g) in enumerate(jobs):
            js = k % 2
            r = DIL[g]
            W = 128 * r
            nchunk = (W + 2048) // W
            ai = h % 2
            nblk = 2048 // W
            ucount = 0
            t_prev = t_accj[ai]
            t_cur = []
            for n in range(nblk):
                for rho in range(r):
                    u4 = nu % 6
                    nu += 1

                    def sl(a0):
                        return slice(a0, a0 + 127 * r + 1, r) if r > 1 else slice(a0, a0 + 128)
                    qsl = sl(n * W + rho)
                    kcur = sl(W + n * W + rho)
                    kprv = sl(n * W + rho)
                    bkS, btS = self.bank()
                    self.mm([(bkS[:, 0:128], kt[js][:, kcur], qt[js][:, qsl], True, True),
                             (bkS[:, 128:256], kt[js][:, kprv], qt[js][:, qsl], True, True)],
                            [t_kt[js], t_qt[js]], [btS])
                    bias = bjf[js] if (Ssup == 0 and n == 0) else bj[js]
                    self.stt("vector", tsb[u4][:], bkS[:, 0:256], 60.0, bias[:], ALU.min, ALU.add,
                             [btS, t_bj[js]], [t_tsb[u4]])
                    self.act(pT[u4][:], tsb[u4][:], AF.Exp, [t_tsb[u4]], [t_pT[u4]])
                    t_u = Trk()
                    t_cur.append(t_u)
                    pend.append((js, u4, vt[js][:, rho * nchunk + n + 1, :], vt[js][:, rho * nchunk + n, :],
                                 acc[ai][:, :, qsl], g == 0, t_prev, t_u))
                    if len(pend) > LAG:
                        stage2(*pend.pop(0))
                    ucount += 1
                    if ucount == LAG + 1 and k + 1 < len(jobs):
                        job_dmas(k + 1)
            t_accj[ai] = t_cur
            if g == 2:
                while pend:
                    stage2(*pend.pop(0))
                t_m = Trk()
                self.act(rcp[:], acc[ai][:, 1, :], AF.Ln, t_cur + [t_m], [t_rcp])
                self.act(rcp[:], rcp[:], AF.Exp, [t_rcp], [t_rcp], scale=-1.0)
                self.tt("gpsimd", mix[:, h, :], acc[ai][:, 0, :], rcp[:], ALU.mult, t_cur + [t_m, t_rcp], [t_mix])
                t_accj[ai] = [t_m]
            if not (g == 2 and h == KC - 1):
                continue
            for q4 in range(4):
                m = NH + Ssup * 4 + q4
                s = 0
                self.load_xt(S["x2s"], m * MT, xt[s], t_xt[s], [self.strk["x2s"][m]])
                for t in range(4):
                    tok0 = q4 * MT + t * 128
                    for half in range(2):
                        bk, bt = self.bank()
                        self.mmacc(bk[:], [(mix[:, hh, tok0:tok0 + 128], wo[:, hh, half * 512:(half + 1) * 512]) for hh in range(KC)],
                                   [t_mix] + t_wo, [bt])
                        zz = xt[s][:, t, half * 512:(half + 1) * 512]
                        self.stt("vector", zz, zz, ALPHA, bk[:], ALU.mult, ALU.add, [t_xt[s][t], bt], [t_xt[s][t]])
                    self.layer_norm(xt[s][:, t, :], t_xt[s][t], lng[:], lnb[:], t_ln, st, mv, t_st)
                mo = Ssup * 4 + q4
                P.dma("gpsimd", S["x3s"][mo * MT:(mo + 1) * MT, :].rearrange("(t p) d -> p t d", p=128), xt[s][:],
                      reads=t_xt[s], writes=[self.strk["x3s"][mo]])


def _t5_bucket(n):
    n = np.asarray(n)
    nf = np.maximum(n, 1).astype(np.float32)
    large = 16 + (np.log(nf / np.float32(16)) / np.float32(np.log(2048 / 16)) * np.float32(16)).astype(np.int32)
    large = np.minimum(large, 31)
    return np.where(n < 16, n, large)


def _static_tables():
    oh = np.zeros((3, 32, 384), np.float32)
    mask = np.full((1, 384), NEG, np.float32)
    for g, r in enumerate(DIL):
        for i in range(384):
            dist = i - 127
            if 0 <= dist <= 128:
                b = int(_t5_bucket(np.array([dist * r]))[0])
                oh[g, b, i] = 1.0
                mask[0, i] = 0.0
    return oh, mask


def make_in_maps(inputs):
    x = np.asarray(inputs["x"], np.float32)
    oh, mask = _static_tables()
    ident = np.eye(128, dtype=np.float32)
    anti = np.ascontiguousarray(ident[::-1])
    sel = np.zeros((NE, NE * 128), np.float32)
    for e in range(NE):
        sel[e, e * 128:(e + 1) * 128] = 1.0
    sel = sel.astype(ml_dtypes.bfloat16)
    shared = {
        "ada_w": np.ascontiguousarray(inputs["ada_w"], np.float32),
        "ada_b": np.ascontiguousarray(inputs["ada_b"], np.float32),
        "ln_g": np.ascontiguousarray(np.broadcast_to(np.asarray(inputs["ln_g"], np.float32).reshape(4, 1, D), (4, 128, D))),
        "ln_b": np.ascontiguousarray(np.broadcast_to(np.asarray(inputs["ln_b"], np.float32).reshape(4, 1, D), (4, 128, D))),
        "conv_w_in": np.ascontiguousarray(inputs["conv_w_in"][0], np.float32),
        "conv_wc": np.ascontiguousarray(np.asarray(inputs["conv_w"][0], np.float32).reshape(3, KC, 128).transpose(2, 1, 0)),
        "conv_w_out": np.ascontiguousarray(inputs["conv_w_out"][0], np.float32),
        "kv_ada_w": np.ascontiguousarray(inputs["kv_ada_w"], np.float32),
        "kv_ada_b": np.ascontiguousarray(np.asarray(inputs["kv_ada_b"], np.float32).reshape(1, 2 * D)),
        "w_kv": np.ascontiguousarray(inputs["w_kv"], np.float32),
        "w_q": np.ascontiguousarray(inputs["attn_w_q"][0], np.float32),
        "w_o": np.ascontiguousarray(inputs["attn_w_o"][0], np.float32),
        "rel_bias": np.ascontiguousarray(inputs["rel_bias"], np.float32),
        "router_w": np.ascontiguousarray(inputs["router_w"], np.float32),
        "router_b": np.ascontiguousarray(np.broadcast_to(np.asarray(inputs["router_bias"], np.float32).reshape(1, NE), (128, NE))),
        "moe_wg": np.ascontiguousarray(inputs["moe_w_gate"], np.float32),
        "moe_wu": np.ascontiguousarray(inputs["moe_w_up"], np.float32),
        "moe_wd": np.ascontiguousarray(inputs["moe_w_down"], np.float32),
        "ident": ident, "antiid": anti, "sel": sel, "oh": oh, "mask": mask,
    }
    maps = []
    for i in range(8):
        b, half = i // 2, i % 2
        own0 = half * OWN
        xin = np.zeros((TOK, D), np.float32)
        xc = np.zeros((128, D), np.float32)
        if half == 1:
            xin[:] = x[b, own0 - NH * MT:own0 + OWN]
            xc[:] = x[b, own0 - NH * MT - 128:own0 - NH * MT]
        else:
            xin[NH * MT:] = x[b, 0:OWN]
        flags = np.zeros((128, 2), np.float32)
        flags[:, 0] = 1.0 if half == 1 else 0.0
        flags[:, 1] = 0.0 if half == 1 else NEG
        m = dict(shared)
        m["xin"] = xin
        m["xc"] = xc
        m["flags"] = flags
        m["c_col"] = np.ascontiguousarray(np.asarray(inputs["c"], np.float32)[b].reshape(KC, 128).T)
        maps.append(m)
    return maps


ALL_PHASES = ["setup", "conv", "moe0", "kv", "q", "attn", "moe1"]
_NC_CACHE = {}


def get_nc(phases=tuple(ALL_PHASES), dbg=()):
    key = (tuple(phases), tuple(dbg))
    if key not in _NC_CACHE:
        _NC_CACHE[key] = Builder(list(phases), dbg).build()
    return _NC_CACHE[key]


def kernel(**inputs):
    nc = get_nc()
    maps = make_in_maps(inputs)
    res = run_bass_kernel_spmd(nc, maps, core_ids=list(range(8)))
    out = np.zeros((4, 2 * OWN, D), np.float32)
    for i in range(8):
        b, half = i // 2, i % 2
        out[b, half * OWN:(half + 1) * OWN] = res.results[i]["out"]
    return out
```
